# Optimizing a Trainium2 kernel written in Bass

```python
import jax, jax.numpy as jnp
from jax import lax
import numpy as np

D_MODEL = 1024
BATCH = 2
SEQ = 8192
DEPTH = 1

D_MIX = D_MODEL
D_CONV = D_MIX // 2
D_ATTN = D_MIX - D_CONV
HEAD_DIM = 64
N_HEADS = D_ATTN // HEAD_DIM
CONV_KERNEL = 31
DILATED_BRANCHES = ((128, 1), (512, 4), (2048, 16))
ROPE_THETA = 10000.0
N_EXPERTS = 32
TOP_K = 4
D_FF = D_MODEL
SWIGLU_LIMIT = 7.0
SWIGLU_ALPHA = 1.702
EXPERT_BLOCK = 128
LN_EPS = 1e-5
DEEPNORM_ALPHA = float((2 * DEPTH) ** 0.25)
DEEPNORM_BETA = float((8 * DEPTH) ** -0.25)
D_IN_PROJ = 2 * D_CONV + 3 * D_ATTN

kernel_name = "hymba_conformer_dilated_moe_deepnorm"


def layer_norm(x, g, b):
    xf = x.astype(jnp.float32)
    mu = jnp.mean(xf, axis=-1, keepdims=True)
    var = jnp.mean(jnp.square(xf - mu), axis=-1, keepdims=True)
    y = (xf - mu) * lax.rsqrt(var + LN_EPS) * g.astype(jnp.float32) + b.astype(jnp.float32)
    return y.astype(x.dtype)


def rope(t, positions):
    e = t.shape[-1]
    inv_freq = ROPE_THETA ** (-jnp.arange(0, e, 2, dtype=jnp.float32) / e)
    ang = positions.astype(jnp.float32)[..., None] * inv_freq
    cos = jnp.concatenate([jnp.cos(ang), jnp.cos(ang)], axis=-1)[:, :, None, :]
    sin = jnp.concatenate([jnp.sin(ang), jnp.sin(ang)], axis=-1)[:, :, None, :]
    t1, t2 = jnp.split(t.astype(jnp.float32), 2, axis=-1)
    rot = jnp.concatenate([-t2, t1], axis=-1)
    return (t.astype(jnp.float32) * cos + rot * sin).astype(t.dtype)


def dilated_branch(q, k, v, window, dilation):
    b, s, h, e = q.shape
    w = window // dilation
    sub_len = s // dilation
    n_blk = -(-sub_len // w)
    pad_len = n_blk * w

    def to_sub(t):
        t = t.reshape(b, sub_len, dilation, h, e).transpose(0, 2, 3, 1, 4)
        t = jnp.pad(t, ((0, 0), (0, 0), (0, 0), (0, pad_len - sub_len), (0, 0)))
        return t.reshape(b, dilation, h, n_blk, w, e)

    def with_prev(t):
        prev = jnp.pad(t, ((0, 0), (0, 0), (0, 0), (1, 0), (0, 0), (0, 0)))[:, :, :, :-1]
        return jnp.concatenate([prev, t], axis=4)

    qs = to_sub(q)
    kb = with_prev(to_sub(k))
    vb = with_prev(to_sub(v))
    scale = 1.0 / np.sqrt(e)
    scores = jnp.einsum('brhnqe,brhnke->brhnqk', qs.astype(jnp.float32),
                        kb.astype(jnp.float32)) * scale
    qi = jnp.arange(w)[:, None]
    ki = jnp.arange(2 * w)[None, :]
    rel = qi + w - ki
    band = (rel >= 0) & (rel <= w)
    blk = jnp.arange(n_blk)[:, None, None]
    mask = band[None] & ((blk > 0) | (ki[None] >= w))
    scores = jnp.where(mask, scores, -jnp.inf)
    m = jnp.max(scores, axis=-1, keepdims=True)
    p = jnp.exp(scores - m)
    den = jnp.sum(p, axis=-1)
    o = jnp.einsum('brhnqk,brhnke->brhnqe', p, vb.astype(jnp.float32)) / den[..., None]
    lse = m[..., 0] + jnp.log(den)
    o = o.reshape(b, dilation, h, pad_len, e)[:, :, :, :sub_len]
    o = o.transpose(0, 3, 1, 2, 4).reshape(b, s, h, e)
    lse = lse.reshape(b, dilation, h, pad_len)[..., :sub_len]
    lse = lse.transpose(0, 3, 1, 2).reshape(b, s, h)
    return o, lse


def hybrid_mixer(x, positions, w_in, conv_w, conv_b, conv_ln_g, conv_ln_b,
                 conv_pw_w, conv_pw_b, w_out):
    b, s, _ = x.shape
    proj = x @ w_in
    splits = np.cumsum([D_CONV, D_CONV, D_ATTN, D_ATTN])
    a, gt, q, k, v = jnp.split(proj, splits, axis=-1)

    u = a * jax.nn.sigmoid(gt)
    u = lax.conv_general_dilated(
        u, conv_w[:, None, :], window_strides=(1,),
        padding=[(CONV_KERNEL - 1, 0)],
        dimension_numbers=('NWC', 'WIO', 'NWC'),
        feature_group_count=D_CONV) + conv_b
    u = jax.nn.silu(layer_norm(u, conv_ln_g, conv_ln_b))
    conv_out = u @ conv_pw_w + conv_pw_b

    q = rope(q.reshape(b, s, N_HEADS, HEAD_DIM), positions)
    k = rope(k.reshape(b, s, N_HEADS, HEAD_DIM), positions)
    v = v.reshape(b, s, N_HEADS, HEAD_DIM)
    outs = []
    lses = []
    for window, dilation in DILATED_BRANCHES:
        o, l = dilated_branch(q, k, v, window, dilation)
        outs.append(o)
        lses.append(l)
    wts = jax.nn.softmax(jnp.stack(lses, axis=0), axis=0)
    attn = jnp.sum(jnp.stack(outs, axis=0) * wts[..., None], axis=0)
    attn_out = attn.reshape(b, s, D_ATTN).astype(x.dtype)

    return jnp.concatenate([conv_out, attn_out], axis=-1) @ w_out


def moe_ffn(x, router_w, router_b, w_gate, b_gate, w_up, b_up, w_down, b_down):
    b, s, dm = x.shape
    xt = x.reshape(-1, dm)
    n_tok = xt.shape[0]
    logits = (xt @ router_w + router_b).astype(jnp.float32)
    top_logit, top_idx = lax.top_k(logits, TOP_K)
    gate = jax.nn.softmax(top_logit, axis=-1)

    n_assign = n_tok * TOP_K
    flat_e = top_idx.reshape(-1)
    flat_tok = jnp.repeat(jnp.arange(n_tok, dtype=jnp.int32), TOP_K)
    flat_g = gate.reshape(-1)
    order = jnp.argsort(flat_e, stable=True)
    sorted_e = flat_e[order]
    counts = jnp.bincount(flat_e, length=N_EXPERTS)
    padded = (counts + EXPERT_BLOCK - 1) // EXPERT_BLOCK * EXPERT_BLOCK
    starts = jnp.cumsum(counts) - counts
    pends = jnp.cumsum(padded)
    pstarts = pends - padded
    dest = pstarts[sorted_e] + (jnp.arange(n_assign) - starts[sorted_e])
    n_rows = (-(-n_assign // EXPERT_BLOCK)) * EXPERT_BLOCK + N_EXPERTS * EXPERT_BLOCK
    n_blocks = n_rows // EXPERT_BLOCK
    buf_tok = jnp.zeros((n_rows,), jnp.int32).at[dest].set(flat_tok[order])
    buf_g = jnp.zeros((n_rows,), jnp.float32).at[dest].set(flat_g[order])
    blk_expert = jnp.minimum(
        jnp.searchsorted(pends, jnp.arange(n_blocks) * EXPERT_BLOCK, side='right'),
        N_EXPERTS - 1)
    xs = xt[buf_tok].reshape(n_blocks, EXPERT_BLOCK, dm)

    def expert_block(args):
        xb, e = args
        g = xb @ w_gate[e] + b_gate[e]
        u = xb @ w_up[e] + b_up[e]
        g = jnp.minimum(g, SWIGLU_LIMIT)
        u = jnp.clip(u, -SWIGLU_LIMIT, SWIGLU_LIMIT)
        hdn = (u + 1.0) * g * jax.nn.sigmoid(SWIGLU_ALPHA * g)
        return hdn @ w_down[e] + b_down[e]

    ys = lax.map(expert_block, (xs, blk_expert)).reshape(n_rows, dm)
    ys = ys.astype(jnp.float32) * buf_g[:, None]
    out = jnp.zeros((n_tok, dm), jnp.float32).at[buf_tok].add(ys)
    return out.astype(x.dtype).reshape(b, s, dm)


def setup_inputs(seed: int = 0) -> dict:
    key = jax.random.key(seed)
    ks = jax.random.split(key, 24)

    def nrm(k, shape, scale):
        return jax.random.normal(k, shape, jnp.float32) * scale

    L = DEPTH
    w_in = nrm(ks[2], (L, D_MODEL, D_IN_PROJ), D_MODEL ** -0.5)
    v_scale = jnp.concatenate([jnp.ones((D_IN_PROJ - D_ATTN,), jnp.float32),
                               jnp.full((D_ATTN,), DEEPNORM_BETA, jnp.float32)])
    w_in = w_in * v_scale
    positions = jnp.broadcast_to(jnp.arange(SEQ, dtype=jnp.int32), (BATCH, SEQ))
    return {
        "x": nrm(ks[0], (BATCH, SEQ, D_MODEL), 1.0),
        "positions": positions,
        "w_in": w_in,
        "conv_w": nrm(ks[3], (L, CONV_KERNEL, D_CONV), CONV_KERNEL ** -0.5),
        "conv_b": nrm(ks[4], (L, D_CONV), 0.02),
        "conv_ln_g": 1.0 + nrm(ks[5], (L, D_CONV), 0.02),
        "conv_ln_b": nrm(ks[6], (L, D_CONV), 0.02),
        "conv_pw_w": nrm(ks[7], (L, D_CONV, D_CONV), D_CONV ** -0.5 * DEEPNORM_BETA),
        "conv_pw_b": nrm(ks[8], (L, D_CONV), 0.02),
        "w_out": nrm(ks[9], (L, D_MIX, D_MODEL), D_MIX ** -0.5 * DEEPNORM_BETA),
        "ln1_g": 1.0 + nrm(ks[10], (L, D_MODEL), 0.02),
        "ln1_b": nrm(ks[11], (L, D_MODEL), 0.02),
        "router_w": nrm(ks[12], (L, D_MODEL, N_EXPERTS), D_MODEL ** -0.5),
        "router_b": nrm(ks[13], (L, N_EXPERTS), 0.01),
        "exp_w_gate": nrm(ks[14], (L, N_EXPERTS, D_MODEL, D_FF), D_MODEL ** -0.5),
        "exp_b_gate": nrm(ks[15], (L, N_EXPERTS, D_FF), 0.02),
        "exp_w_up": nrm(ks[16], (L, N_EXPERTS, D_MODEL, D_FF), D_MODEL ** -0.5),
        "exp_b_up": nrm(ks[17], (L, N_EXPERTS, D_FF), 0.02),
        "exp_w_down": nrm(ks[18], (L, N_EXPERTS, D_FF, D_MODEL), D_FF ** -0.5 * DEEPNORM_BETA),
        "exp_b_down": nrm(ks[19], (L, N_EXPERTS, D_MODEL), 0.02),
        "ln2_g": 1.0 + nrm(ks[20], (L, D_MODEL), 0.02),
        "ln2_b": nrm(ks[21], (L, D_MODEL), 0.02),
    }


def reference(x, positions, w_in, conv_w, conv_b, conv_ln_g, conv_ln_b, conv_pw_w,
              conv_pw_b, w_out, ln1_g, ln1_b, router_w, router_b, exp_w_gate,
              exp_b_gate, exp_w_up, exp_b_up, exp_w_down, exp_b_down, ln2_g, ln2_b):
    for l in range(DEPTH):
        mix = hybrid_mixer(x, positions, w_in[l], conv_w[l], conv_b[l], conv_ln_g[l],
                           conv_ln_b[l], conv_pw_w[l], conv_pw_b[l], w_out[l])
        x = layer_norm(DEEPNORM_ALPHA * x + mix, ln1_g[l], ln1_b[l])
        ffn = moe_ffn(x, router_w[l], router_b[l], exp_w_gate[l], exp_b_gate[l],
                      exp_w_up[l], exp_b_up[l], exp_w_down[l], exp_b_down[l])
        x = layer_norm(DEEPNORM_ALPHA * x + ffn, ln2_g[l], ln2_b[l])
    return x
```

```python
import math
import numpy as np
import ml_dtypes
from contextlib import ExitStack
import concourse.bass as bass
import concourse.mybir as mybir
from concourse.bass_utils import run_bass_kernel_spmd

F32 = mybir.dt.float32
BF16 = mybir.dt.bfloat16
I32 = mybir.dt.int32
ALU = mybir.AluOpType
AF = mybir.ActivationFunctionType
AX = mybir.AxisListType

NCORES = 8
TOK = 2048
TT = 4096
DM = 1024
NE = 32
CAP = 384
NSLOT = NE * CAP
ALPHA = float(2.0 ** 0.25)
LN_EPS = 1e-5
PI = math.pi
ENG = ['pe', 'act', 'dve', 'pool', 'sp']


class Res:
    __slots__ = ('name', 'w', 'r')

    def __init__(self, name):
        self.name = name
        self.w = None
        self.r = []


class Sched:
    def __init__(self, nc, stack):
        self.nc = nc
        self.stack = stack
        self.ops = {e: [] for e in ENG}
        self.cnt = {}
        self.seen = {e: {} for e in ENG}
        self.sems = {}
        self.clk = {}
        self.nres = 0

    def res(self, name=None):
        self.nres += 1
        return Res(name or f"r{self.nres}")

    def _tok_kv(self, tok):
        if tok[0] == 'd':
            return tok[1], tok[2]
        _, eng, idx = tok
        ops = self.ops[eng]
        for j in range(idx, len(ops)):
            s = ops[j][2]
            if s is not None and s[0] == eng:
                return eng, s[2]
        j = len(ops) - 1
        while ops[j][3] != 'c':
            j -= 1
        assert j >= idx
        v = self.cnt.get(eng, 0) + 1
        self.cnt[eng] = v
        ops[j][2] = [eng, 1, v]
        self.clk[(eng, v)] = ops[j][4]
        return eng, v

    def _need(self, eng, waits, tok, raw):
        if tok is None:
            return
        if tok[0] == 'c' and tok[1] == eng and not raw:
            return
        k, v = self._tok_kv(tok)
        if self.seen[eng].get(k, 0) >= v:
            return
        if waits.get(k, 0) < v:
            waits[k] = v

    def _commit_waits(self, eng, waits):
        seen = self.seen[eng]
        for k, v in waits.items():
            if seen.get(k, 0) < v:
                seen[k] = v
            c = self.clk.get((k, v))
            if c:
                for k2, v2 in c.items():
                    if seen.get(k2, 0) < v2:
                        seen[k2] = v2

    def op(self, eng, fn, reads=(), writes=(), acc=False):
        waits = {}
        for r in reads:
            self._need(eng, waits, r.w, True)
        if not acc:
            for w in writes:
                self._need(eng, waits, w.w, False)
                for t in w.r:
                    self._need(eng, waits, t, False)
        self._commit_waits(eng, waits)
        snap = dict(self.seen[eng])
        idx = len(self.ops[eng])
        self.ops[eng].append([waits, fn, None, 'c', snap])
        tok = ('c', eng, idx)
        for r in reads:
            r.r.append(tok)
        for w in writes:
            w.w = tok
            if not acc:
                w.r = []
        return tok

    def dma(self, eng, fn, sem, reads=(), writes=(), nowaw=False):
        waits = {}
        for r in reads:
            self._need(eng, waits, r.w, True)
        for w in writes:
            if not (nowaw == 'any' or (nowaw and w.w is not None and w.w[0] == 'd' and w.w[1] == sem)):
                self._need(eng, waits, w.w, True)
            for t in w.r:
                self._need(eng, waits, t, True)
        self._commit_waits(eng, waits)
        v = self.cnt.get(sem, 0) + 16
        self.cnt[sem] = v
        snap = dict(self.seen[eng])
        self.clk[(sem, v)] = snap
        self.ops[eng].append([waits, fn, [sem, 16, v], 'd', snap])
        tok = ('d', sem, v, eng)
        for r in reads:
            r.r.append(tok)
        for w in writes:
            w.w = tok
            w.r = []
        return tok

    def wait_all(self, eng, toks):
        waits = {}
        for t in toks:
            self._need(eng, waits, t, True)
        self._commit_waits(eng, waits)
        self.ops[eng].append([waits, None, None, 'w', None])

    def barrier(self, exclude=()):
        toks = []
        for e in ENG:
            ops = self.ops[e]
            for j in range(len(ops) - 1, -1, -1):
                if ops[j][3] == 'c':
                    toks.append(('c', e, j))
                    break
        for k, v in list(self.cnt.items()):
            if k not in ENG and not any(k.startswith(x) for x in exclude):
                toks.append(('d', k, v, None))
        for e in ENG:
            self.wait_all(e, toks)

    def emit(self):
        nc = self.nc
        for k in sorted(self.cnt.keys()):
            self.sems[k] = self.stack.enter_context(nc.semaphore("s_" + k))
        engs = {'pe': 'tensor', 'act': 'scalar', 'dve': 'vector', 'pool': 'gpsimd', 'sp': 'sync'}
        sems = self.sems
        with nc.Block() as block:
            for e in ENG:
                ops = self.ops[e]

                def body(engine, ops=ops):
                    for waits, fn, sig, kind, _ in ops:
                        for k, v in waits.items():
                            engine.wait_ge(sems[k], v)
                        if fn is None:
                            continue
                        ins = fn(engine)
                        if sig is not None:
                            ins.then_inc(sems[sig[0]], sig[1])
                getattr(block, engs[e])(body)


_REG = {}


def bcreg(e):
    if 'bc' not in _REG:
        _REG['bc'] = e.to_reg(NSLOT - 1)
    return _REG['bc']


def strided(ap, start, step, n):
    pat = [list(x) for x in ap.ap]
    es = pat[-1][0]
    return bass.AP(ap.tensor, ap.offset + start * es, pat[:-1] + [[step * es, n]])


def build_program(debug=None, stop=None):
    debug = debug or []
    nc = bass.Bass("TRN2", target_bir_lowering=False)
    _REG.clear()
    st = ExitStack()
    S = Sched(nc, st)

    def din(name, shape, dt=F32):
        return nc.dram_tensor(name, list(shape), dt, kind="ExternalInput").ap()

    xT = din("xT", [8, 128, 8 * 512])
    xtok = din("xtok", [TOK, DM])
    posd = din("pos", [1, TT], I32)
    w_in = din("w_in", [DM, 2560])
    convw = din("convw", [128, 4 * 31])
    cvec = din("cvec", [128, 16])
    pw_w = din("pw_w", [512, 512])
    w_out = din("w_out", [DM, DM])
    lnvec = din("lnvec", [128, 4 * DM])
    rw = din("rw", [DM, NE])
    rbr = din("rbr", [128, NE])
    if stop:
        wg_d = wu_d = wd_d = None
    else:
        wg_d = din("wg", [NE * 128, 8 * DM])
        wu_d = din("wu", [NE * 128, 8 * DM])
        wd_d = din("wd", [NE * 128, 8 * DM])
    bgu = din("bgu", [128, 2 * NE * 8])
    bd_d = din("bd", [NE, DM])
    cst = din("cst", [128, 4])
    zsrc = din("zsrc", [256, DM], BF16)
    out_d = nc.dram_tensor("out", [TOK, DM], F32, kind="ExternalOutput").ap()
    xs_d = nc.dram_tensor("xs_scr", [NSLOT, DM], BF16, kind="Internal").ap()
    ys_d = nc.dram_tensor("ys_scr", [NSLOT, DM], F32, kind="Internal").ap()
    x1_d = nc.dram_tensor("x1_scr", [TOK, DM], F32, kind="Internal").ap()
    dbg_out = {}

    ARENA = 207 * 1024
    with st:
        A = st.enter_context(nc.sbuf_tensor("arena", [128, ARENA // 2], BF16))
        PS = st.enter_context(nc.psum_tensor("PS", [128, 4096], F32))
        PSb = PS[:].bitcast(BF16)

        cur = [0]

        def alloc(shape, dt, name=None):
            n = int(np.prod(shape))
            nb = n * mybir.dt.size(dt)
            nb = (nb + 63) // 64 * 64
            off = cur[0]
            cur[0] += nb
            assert cur[0] <= ARENA, (name, cur[0])
            ap = A[:, off // 2: off // 2 + n * mybir.dt.size(dt) // 2]
            if dt != BF16:
                ap = ap.bitcast(dt)
            if len(shape) == 2:
                ap = ap.rearrange("p (a b) -> p a b", b=shape[1])
            elif len(shape) == 3:
                ap = ap.rearrange("p (a b c) -> p a b c", b=shape[1], c=shape[2])
            return ap

        def bank(b):
            return PS[:, 512 * b:512 * b + 512]

        def bankb(b):
            return PSb[:, 1024 * b:1024 * b + 1024]
        rB = [S.res(f"bank{b}") for b in range(8)]

        ndbg = [0]

        def dump(name, ap, res, shape, dt=F32):
            if name not in debug:
                return
            t = nc.dram_tensor("dbg_" + name, list(shape), dt, kind="ExternalOutput").ap()
            dbg_out[name] = t
            ndbg[0] += 1
            S.dma('sp', lambda e: e.dma_start(out=t, in_=ap), f'dbg{ndbg[0]}', reads=res)

        identf = alloc([128], F32)
        identb = alloc([128], BF16)
        onesb = alloc([128], BF16)
        onesf = alloc([128], F32)
        trib = alloc([128], BF16)
        M1 = alloc([512], BF16)
        M1f = alloc([512], BF16)
        M2f = alloc([512], BF16)
        M3 = alloc([4, 512], BF16)
        cstt = alloc([4], F32)
        ecf = alloc([NE], F32)
        r_const = S.res("const")
        r_idx = S.res("idx")
        mcf = alloc([128], F32)
        mpf_ = alloc([128], F32)
        mpff = alloc([128], F32)
        eci = alloc([NE], I32)
        xt_mark = cur[0]
        XT = alloc([8, 8, 512], BF16, "XT")
        rXT = [S.res(f"xt{i}") for i in range(8)]
        attnT_mark = cur[0]
        attnT = alloc([4, TOK], BF16, "attnT")
        r_attnT = [S.res(f"attnT{g}") for g in range(4)]
        persist_mark = cur[0]

        for i in range(8):
            S.dma('pool', lambda e, i=i: e.dma_start(out=XT[:, i].rearrange("p k t -> p (k t)"), in_=xT[i], max_dma_last_dim=8192),
                  f'ldxt{i}', writes=[rXT[i]])
        S.dma('sp', lambda e: e.dma_start(out=cstt, in_=cst), 'ldc', writes=[r_const])

        def aff(out, pattern, cm, op):
            S.op('pool', lambda e: e.memset(out, 1.0), writes=[r_const])
            S.op('pool', lambda e: e.affine_select(out=out, in_=out, pattern=pattern, compare_op=op, fill=0.0,
                                                   base=0, channel_multiplier=cm), reads=[r_const], writes=[r_const])
        aff(identf, [[-1, 128]], 1, ALU.is_equal)
        aff(mcf, [[1, 128]], -1, ALU.is_ge)
        aff(mpf_, [[-1, 128]], 1, ALU.is_ge)
        S.op('pool', lambda e: e.tensor_copy(out=identb, in_=identf), reads=[r_const], writes=[r_const])
        S.op('pool', lambda e: e.memset(onesb, 1.0), writes=[r_const])
        S.op('pool', lambda e: e.memset(onesf, 1.0), writes=[r_const])
        S.op('pool', lambda e: e.tensor_tensor(out=trib, in0=mcf, in1=identf, op=ALU.subtract), reads=[r_const], writes=[r_const])
        S.op('dve', lambda e: e.tensor_scalar(out=mpff, in0=mpf_, scalar1=cstt[:, 2:3], scalar2=None, op0=ALU.mult),
             reads=[r_const], writes=[r_const])
        for (M, parts) in ((M1, [mpf_, mcf, mpf_, mcf]), (M1f, [mpff, mcf, mpf_, mcf]), (M2f, [mpff, mcf, mpff, mcf])):
            for s_, src in enumerate(parts):
                S.op('pool', lambda e, M=M, s_=s_, src=src: e.tensor_copy(out=M[:, 128 * s_:128 * s_ + 128], in_=src),
                     reads=[r_const], writes=[r_const])
        for tb in range(4):
            for r_ in range(8):
                S.op('pool', lambda e, tb=tb, r_=r_: e.tensor_copy(out=M3[:, tb, 64 * r_:64 * r_ + 32], in_=mpff[:, 32 * tb:32 * tb + 32]),
                     reads=[r_const], writes=[r_const])
                S.op('pool', lambda e, tb=tb, r_=r_: e.tensor_copy(out=M3[:, tb, 64 * r_ + 32:64 * r_ + 64], in_=mcf[:, 32 * tb:32 * tb + 32]),
                     reads=[r_const], writes=[r_const])
        S.op('pool', lambda e: e.iota(eci, pattern=[[CAP, NE]], base=0, channel_multiplier=0), writes=[r_const])
        S.op('pool', lambda e: e.tensor_copy(out=ecf, in_=eci), reads=[r_const], writes=[r_const])
        r_xs = S.res("xs_scr")

        cosT = alloc([TT], F32, "cos")
        sinT = alloc([TT], F32, "sin")
        r_tab = S.res("tab")
        wgt = alloc([8, 5, 128], BF16, "wgt")
        r_wgt = S.res("wgt")
        QT = alloc([TOK], BF16)
        KT = alloc([TT], BF16)
        VT = alloc([TT], BF16)
        r_QT = [S.res() for _ in range(4)]
        r_KT = [S.res() for _ in range(8)]
        r_VT = [S.res() for _ in range(8)]
        NKB = 69
        VTOK = alloc([NKB, 192], BF16)
        r_vtok = S.res("vtok")
        tmp_mark = cur[0]
        Ebuf = [alloc([512], BF16) for _ in range(3)]
        r_E = [S.res() for _ in range(3)]
        PTb = [alloc([512], BF16) for _ in range(3)]
        r_PT = [S.res() for _ in range(3)]
        rt = [alloc([512], F32) for _ in range(4)]
        r_rt = [S.res() for _ in range(4)]
        lnb = [alloc([512], F32) for _ in range(2)]
        r_lnb = [S.res() for _ in range(2)]
        rdb = [alloc([512], F32) for _ in range(2)]
        r_rdb = [S.res() for _ in range(2)]
        att_mark = cur[0]

        cur[0] = tmp_mark
        posi = alloc([512], I32)
        ang = alloc([512], F32)
        a2 = alloc([512], F32)
        kfi = alloc([512], I32)
        kff = alloc([512], F32)
        rr = alloc([512], F32)
        r_tmp = S.res("tabtmp")
        S.op('pool', lambda e: e.memset(VTOK[:, :, 64:128], 1.0), writes=[r_vtok])
        for i in range(8):
            sl = slice(512 * i, 512 * i + 512)
            S.dma('sp', lambda e, sl=sl: e.dma_start(out=posi, in_=posd[:, sl].partition_broadcast(128)), 'ldpos', writes=[r_tmp])
            S.op('dve', lambda e: e.tensor_copy(out=ang, in_=posi), reads=[r_tmp], writes=[r_tmp])
            S.op('dve', lambda e: e.tensor_scalar(out=ang, in0=ang, scalar1=cstt[:, 0:1], scalar2=None, op0=ALU.mult),
                 reads=[r_tmp, r_const], writes=[r_tmp])
            for (tab, shift, scl) in ((sinT, 0.0, cstt[:, 1:2]), (cosT, PI / 2, 1.0)):
                S.op('dve', lambda e, shift=shift: e.tensor_scalar(out=a2, in0=ang, scalar1=shift, scalar2=None, op0=ALU.add),
                     reads=[r_tmp], writes=[r_tmp])
                S.op('dve', lambda e: e.tensor_scalar(out=kfi, in0=a2, scalar1=1.0 / (2 * PI), scalar2=None, op0=ALU.mult),
                     reads=[r_tmp], writes=[r_tmp])
                S.op('dve', lambda e: e.tensor_copy(out=kff, in_=kfi), reads=[r_tmp], writes=[r_tmp])
                S.op('dve', lambda e: e.scalar_tensor_tensor(out=rr, in0=kff, scalar=-2 * PI, in1=a2, op0=ALU.mult, op1=ALU.add),
                     reads=[r_tmp], writes=[r_tmp])
                S.op('dve', lambda e: e.tensor_scalar(out=rr, in0=rr, scalar1=-3.14159, scalar2=3.14159, op0=ALU.max, op1=ALU.min),
                     reads=[r_tmp], writes=[r_tmp])
                S.op('act', lambda e, tab=tab, sl=sl, scl=scl: e.activation(out=tab[:, sl], in_=rr, func=AF.Sin, scale=scl),
                     reads=[r_tmp, r_const], writes=[r_tab])
        S.barrier(exclude=('ldxt',))
        cur[0] = att_mark

        w_in_v = w_in.rearrange("(k p) n -> p k n", p=128)

        def kcols(branch, r_, j):
            if branch == 1:
                return (2048 + 128 * j, 1)
            if branch == 2:
                return (2048 + 512 * j + r_, 4)
            return (2048 + 2048 * j + r_, 16)

        def vidx(branch, r_, j):
            if branch == 1:
                return j + 1
            if branch == 2:
                return 17 + r_ * 5 + (j + 1)
            return 37 + r_ * 2 + (j + 1)

        ucount = [0]
        for g in range(4):
            q0, k0, v0 = 1024 + 128 * g, 1536 + 128 * g, 2048 + 128 * g
            for (slot, c0) in ((0, q0), (2, k0), (4, v0)):
                S.dma('pool', lambda e, slot=slot, c0=c0: e.dma_start(out=wgt[:, :, slot, :], in_=w_in_v[:, :, c0:c0 + 128]),
                      'ldwg', writes=[r_wgt], nowaw=True)
            for (slot, c0) in ((1, q0), (3, k0)):
                for hh in range(2):
                    for half in range(2):
                        d0 = 64 * hh + 32 * half
                        s0 = c0 + 64 * hh + 32 * (1 - half)
                        S.dma('pool', lambda e, slot=slot, d0=d0, s0=s0: e.dma_start(out=wgt[:, :, slot, d0:d0 + 32], in_=w_in_v[:, :, s0:s0 + 32]),
                              'ldwg', writes=[r_wgt], nowaw=True)

            def proj(slot, b, i):
                for kc in range(8):
                    S.op('pe', lambda e, kc=kc: e.matmul(bank(b), lhsT=wgt[:, kc, slot, :], rhs=XT[:, i, kc, :],
                                                       start=(kc == 0), stop=(kc == 7)),
                         reads=[r_wgt, rXT[i]], writes=[rB[b]], acc=(kc > 0))

            pc = [0]

            def rope(dst, rdst, bA, bB, i):
                sl = slice(512 * i, 512 * i + 512)
                a = pc[0] % 2
                pc[0] += 1
                t1, t2 = rt[2 * a], rt[2 * a + 1]
                S.op('dve', lambda e: e.tensor_tensor(out=t1, in0=bank(bA), in1=cosT[:, sl], op=ALU.mult),
                     reads=[rB[bA], r_tab], writes=[r_rt[2 * a]])
                S.op('dve', lambda e: e.tensor_tensor(out=t2, in0=bank(bB), in1=sinT[:, sl], op=ALU.mult),
                     reads=[rB[bB], r_tab], writes=[r_rt[2 * a + 1]])
                S.op('pool', lambda e: e.tensor_tensor(out=dst, in0=t1, in1=t2, op=ALU.add),
                     reads=[r_rt[2 * a], r_rt[2 * a + 1]], writes=[rdst])

            for i in range(8):
                pb = 3 * (i % 2)
                proj(2, pb, i)
                proj(3, pb + 1, i)
                rope(KT[:, 512 * i:512 * i + 512], r_KT[i], pb, pb + 1, i)
                proj(4, pb + 2, i)
                S.op('act', lambda e, i=i, pb=pb: e.copy(out=VT[:, 512 * i:512 * i + 512], in_=bank(pb + 2)),
                     reads=[rB[pb + 2]], writes=[r_VT[i]])
                if i >= 4:
                    proj(0, 6, i)
                    proj(1, 7, i)
                    rope(QT[:, 512 * (i - 4):512 * (i - 4) + 512], r_QT[i - 4], 6, 7, i)
            if g == 0:
                dump("KT0", KT, r_KT, [128, TT], BF16)
                dump("QT0", QT, r_QT, [128, TOK], BF16)
                dump("VT0", VT, r_VT, [128, TT], BF16)

            kbs = []
            for j in range(-1, 16):
                kbs.append((1, 0, j))
            for r_ in range(4):
                for j in range(-1, 4):
                    kbs.append((2, r_, j))
            for r_ in range(16):
                for j in range(-1, 1):
                    kbs.append((3, r_, j))
            assert len(kbs) == NKB
            for c0 in range(0, NKB, 8):
                chunk = kbs[c0:c0 + 8]
                b = 6 + (c0 // 8) % 2
                for s_, (br, r_, j) in enumerate(chunk):
                    stt, stp = kcols(br, r_, j)
                    assert vidx(br, r_, j) == c0 + s_
                    S.op('pe', lambda e, b=b, s_=s_, stt=stt, stp=stp: e.transpose(bankb(b)[:, 128 * s_:128 * s_ + 128], strided(VT, stt, stp, 128), identb),
                         reads=r_VT + [r_const], writes=[rB[b]], acc=(s_ > 0))
                n = len(chunk)
                src = bankb(b)[:, 0:128 * n].rearrange("p (n f) -> p n f", f=128)
                S.op('act', lambda e, c0=c0, n=n, src=src: e.copy(out=VTOK[:, c0:c0 + n, 0:64], in_=src[:, :, 0:64]),
                     reads=[rB[b]], writes=[r_vtok])
                S.op('dve', lambda e, c0=c0, n=n, src=src: e.tensor_copy(out=VTOK[:, c0:c0 + n, 128:192], in_=src[:, :, 64:128]),
                     reads=[rB[b]], writes=[r_vtok])

            units = []
            for tb in range(4):
                for hh in range(2):
                    hs = slice(64 * hh, 64 * hh + 64)
                    KTh, QTh = KT[hs, :], QT[hs, :]
                    ob = 4 + (tb * 2 + hh) % 2
                    O = bank(ob)
                    ulist = []
                    for half in range(2):
                        smm, pv = [], []
                        for s_ in range(2):
                            qb = 4 * tb + 2 * half + s_
                            for w_, j in enumerate((qb - 1, qb)):
                                ks, kp = kcols(1, 0, j)
                                col = 256 * s_ + 128 * w_
                                smm.append((strided(KTh, ks, kp, 128), QTh[:, 128 * qb:128 * qb + 128], col, 128))
                                pv.append((vidx(1, 0, j), col, 128, O[:, 128 * (qb - 4 * tb):128 * (qb - 4 * tb) + 128]))
                        mask = M1f if (tb == 0 and half == 0) else M1
                        ulist.append((smm, mask, pv))
                    for half in range(2):
                        smm, pv = [], []
                        for s_ in range(2):
                            r_ = 2 * half + s_
                            for w_, j in enumerate((tb - 1, tb)):
                                ks, kp = kcols(2, r_, j)
                                col = 256 * s_ + 128 * w_
                                smm.append((strided(KTh, ks, kp, 128), strided(QTh, 512 * tb + r_, 4, 128), col, 128))
                                pv.append((vidx(2, r_, j), col, 128, strided(O, r_, 4, 128)))
                        mask = M2f if tb == 0 else M1
                        ulist.append((smm, mask, pv))
                    for half in range(2):
                        smm, pv = [], []
                        for s_ in range(8):
                            r_ = 8 * half + s_
                            for w_, j in enumerate((-1, 0)):
                                ks, kp = kcols(3, r_, j)
                                col = 64 * s_ + 32 * w_
                                smm.append((strided(KTh, ks, kp, 128), strided(QTh, 512 * tb + r_, 16, 32), col, 32))
                                pv.append((vidx(3, r_, j), col, 32, strided(O, r_, 16, 32)))
                        ulist.append((smm, M3[:, tb, :], pv))
                    for ui, u in enumerate(ulist):
                        units.append((tb, hh, ob, ui, u))

            def emit_S(un, sb, eb):
                tb, hh, ob, ui, (smm, mask, pv) = un
                for n_, (lh, rh, col, N) in enumerate(smm):
                    S.op('pe', lambda e, lh=lh, rh=rh, col=col, N=N: e.matmul(bank(sb)[:, col:col + N], lhsT=lh, rhs=rh, start=True, stop=True,
                                                                              skip_group_check=True),
                         reads=r_KT + r_QT, writes=[rB[sb]], acc=(n_ > 0))
                S.op('act', lambda e: e.activation(out=Ebuf[eb], in_=bank(sb), func=AF.Exp, scale=0.125),
                     reads=[rB[sb]], writes=[r_E[eb]])
                eng = 'dve' if (ucount[0] % 3 != 2) else 'pool'
                S.op(eng, lambda e: e.tensor_tensor(out=PTb[eb], in0=Ebuf[eb], in1=mask, op=ALU.mult),
                     reads=[r_E[eb], r_const], writes=[r_PT[eb]])

            def emit_PV(un, eb):
                tb, hh, ob, ui, (smm, mask, pv) = un
                for n_, (vi, col, N, oap) in enumerate(pv):
                    first = (ui == 0 and n_ == 0)
                    last = (ui == 5 and n_ == len(pv) - 1)
                    lh = VTOK[:, vi, 0:128] if hh == 0 else VTOK[:, vi, 64:192]
                    S.op('pe', lambda e, lh=lh, col=col, N=N, oap=oap, first=first, last=last: e.matmul(
                        oap, lhsT=lh, rhs=PTb[eb][:, col:col + N], start=first, stop=last, skip_group_check=True),
                        reads=[r_vtok, r_PT[eb]], writes=[rB[ob]], acc=(not first))
                if ui == 5:
                    num = slice(0, 64) if hh == 0 else slice(64, 128)
                    den = slice(64, 128) if hh == 0 else slice(0, 64)
                    a = (tb * 2 + hh) % 2
                    dsta = attnT[num, g, 512 * tb:512 * tb + 512]
                    S.op('act', lambda e: e.activation(out=lnb[a][den, :], in_=bank(ob)[den, :], func=AF.Ln),
                         reads=[rB[ob]], writes=[r_lnb[a]])
                    S.op('act', lambda e: e.activation(out=rdb[a][den, :], in_=lnb[a][den, :], func=AF.Exp, scale=-1.0),
                         reads=[r_lnb[a]], writes=[r_rdb[a]])
                    S.op('dve', lambda e: e.tensor_tensor(out=dsta, in0=bank(ob)[num, :], in1=rdb[a][den, :], op=ALU.mult),
                         reads=[rB[ob], r_rdb[a]], writes=[r_attnT[g]])

            nun = len(units)
            for u in range(nun + 1):
                if u < nun:
                    emit_S(units[u], u % 4, u % 3)
                    ucount[0] += 1
                if u >= 1:
                    emit_PV(units[u - 1], (u - 1) % 3)
        dump("attnT", attnT, r_attnT, [128, 4, TOK], BF16)
        S.barrier()
        cur[0] = persist_mark

        convoutT = alloc([4, TOK], BF16, "convoutT")
        r_convout = S.res("convout")
        conv_mark = cur[0]
        uT = alloc([4, TOK + 32], BF16)
        r_uT = [S.res() for _ in range(5)]
        diag = alloc([124, 128], BF16)
        r_diag = S.res()
        cwt = alloc([124], F32)
        cvt = alloc([16], F32)
        r_cv = S.res()
        pwb = alloc([4, 512], BF16)
        r_pwb = S.res()
        r_cT = S.res()
        r_csq = S.res()
        sg = [alloc([512], F32) for _ in range(2)]
        r_sg = [S.res() for _ in range(2)]
        mean = alloc([512], F32)
        msq = alloc([512], F32)
        var = alloc([512], F32)
        rstd = alloc([512], F32)
        r_st = S.res()
        tn = [alloc([512], F32) for _ in range(2)]
        r_tn = [S.res() for _ in range(2)]
        sT = alloc([4, 512], BF16)
        r_sT = S.res()
        wag_mark = cur[0]
        wag = alloc([8, 1024], BF16)
        r_wag = S.res()
        cur[0] = wag_mark
        cT = alloc([4, 512], F32)
        csq = alloc([4, 512], F32)

        for h_ in range(2):
            S.dma('pool', lambda e, h_=h_: e.dma_start(out=wag[:, :, 512 * h_:512 * h_ + 512], in_=w_in_v[:, :, 512 * h_:512 * h_ + 512]),
                  'ldwag', writes=[r_wag], nowaw=True)
        S.dma('sp', lambda e: e.dma_start(out=cwt, in_=convw), 'ldcw', writes=[r_cv])
        S.dma('sp', lambda e: e.dma_start(out=cvt, in_=cvec), 'ldcv', writes=[r_cv], nowaw=True)
        S.dma('pool', lambda e: e.dma_start(out=pwb, in_=pw_w.rearrange("(k p) n -> p k n", p=128)), 'ldpw', writes=[r_pwb])
        for c in range(4):
            for j in range(31):
                n_ = c * 31 + j
                S.op('dve', lambda e, n_=n_: e.tensor_scalar(out=diag[:, n_, :], in0=identf, scalar1=cwt[:, n_:n_ + 1], scalar2=None, op0=ALU.mult),
                     reads=[r_cv, r_const], writes=[r_diag])

        def glu(xcols, ucols, ncol, ru, xi):
            for c in range(4):
                bA, bG = 2 * (c % 2), 2 * (c % 2) + 1
                for kc in range(8):
                    S.op('pe', lambda e, kc=kc, c=c, bA=bA: e.matmul(bank(bA)[:, 0:ncol], lhsT=wag[:, kc, 128 * c:128 * c + 128], rhs=XT[:, xi, kc, xcols],
                                                                    start=(kc == 0), stop=(kc == 7)),
                         reads=[r_wag, rXT[xi]], writes=[rB[bA]], acc=(kc > 0))
                for kc in range(8):
                    S.op('pe', lambda e, kc=kc, c=c, bG=bG: e.matmul(bank(bG)[:, 0:ncol], lhsT=wag[:, kc, 512 + 128 * c:512 + 128 * c + 128], rhs=XT[:, xi, kc, xcols],
                                                                    start=(kc == 0), stop=(kc == 7)),
                         reads=[r_wag, rXT[xi]], writes=[rB[bG]], acc=(kc > 0))
                a = c % 2
                S.op('act', lambda e, a=a, bG=bG: e.activation(out=sg[a][:, 0:ncol], in_=bank(bG)[:, 0:ncol], func=AF.Sigmoid),
                     reads=[rB[bG]], writes=[r_sg[a]])
                S.op('dve', lambda e, a=a, bA=bA, c=c: e.tensor_tensor(out=uT[:, c, ucols], in0=bank(bA)[:, 0:ncol], in1=sg[a][:, 0:ncol], op=ALU.mult),
                     reads=[rB[bA], r_sg[a]], writes=[ru])
        glu(slice(480, 512), slice(0, 32), 32, r_uT[0], 3)
        for t in range(4):
            glu(slice(0, 512), slice(32 + 512 * t, 32 + 512 * t + 512), 512, r_uT[t + 1], 4 + t)
        dump("uT", uT, r_uT, [128, 4, TOK + 32], BF16)
        xs_z = xs_d.rearrange("(n r) d -> n r d", r=256)
        for n_ in range(NSLOT // 256):
            S.dma('sp', lambda e, n_=n_: e.dma_start(out=xs_z[n_], in_=zsrc), 'zxs', writes=[r_xs], nowaw=True)

        for t in range(4):
            ureads = [r_uT[t], r_uT[t + 1]] if t > 0 else [r_uT[0], r_uT[1]]
            for c in range(4):
                b = 4 + c % 2
                for j in range(31):
                    S.op('pe', lambda e, c=c, j=j, b=b, t=t: e.matmul(bank(b), lhsT=diag[:, c * 31 + j, :], rhs=uT[:, c, 512 * t + 2 + j:512 * t + 2 + j + 512],
                                                                     start=(j == 0), stop=(j == 30)),
                         reads=[r_diag] + ureads, writes=[rB[b]], acc=(j > 0))
                S.op('act', lambda e, c=c, b=b: e.activation(out=cT[:, c, :], in_=bank(b), func=AF.Identity, bias=cvt[:, c:c + 1]),
                     reads=[rB[b], r_cv], writes=[r_cT])
                S.op('act', lambda e, c=c: e.activation(out=csq[:, c, :], in_=cT[:, c, :], func=AF.Square),
                     reads=[r_cT], writes=[r_csq])
            if t == 0:
                dump("cT0", cT, [r_cT], [128, 4, 512])
            for c in range(4):
                S.op('pe', lambda e, c=c: e.matmul(bank(6), lhsT=onesf, rhs=cT[:, c, :], start=(c == 0), stop=(c == 3)),
                     reads=[r_cT, r_const], writes=[rB[6]], acc=(c > 0))
            for c in range(4):
                S.op('pe', lambda e, c=c: e.matmul(bank(7), lhsT=onesf, rhs=csq[:, c, :], start=(c == 0), stop=(c == 3)),
                     reads=[r_csq, r_const], writes=[rB[7]], acc=(c > 0))
            S.op('act', lambda e: e.activation(out=mean, in_=bank(6), func=AF.Copy, scale=1.0 / 512), reads=[rB[6]], writes=[r_st])
            S.op('dve', lambda e: e.tensor_tensor(out=msq, in0=mean, in1=mean, op=ALU.mult), reads=[r_st], writes=[r_st])
            S.op('dve', lambda e: e.scalar_tensor_tensor(out=var, in0=bank(7), scalar=1.0 / 512, in1=msq, op0=ALU.mult, op1=ALU.subtract),
                 reads=[rB[7], r_st], writes=[r_st])
            S.op('dve', lambda e: e.tensor_scalar(out=var, in0=var, scalar1=LN_EPS, scalar2=None, op0=ALU.add), reads=[r_st], writes=[r_st])
            S.op('act', lambda e: e.activation(out=var, in_=var, func=AF.Ln), reads=[r_st], writes=[r_st])
            S.op('act', lambda e: e.activation(out=rstd, in_=var, func=AF.Exp, scale=-0.5), reads=[r_st], writes=[r_st])
            for c in range(4):
                a = c % 2
                S.op('dve', lambda e, c=c, a=a: e.tensor_tensor(out=tn[a], in0=cT[:, c, :], in1=mean, op=ALU.subtract),
                     reads=[r_cT, r_st], writes=[r_tn[a]])
                S.op('pool', lambda e, a=a: e.tensor_tensor(out=tn[a], in0=tn[a], in1=rstd, op=ALU.mult),
                     reads=[r_tn[a], r_st], writes=[r_tn[a]])
                S.op('act', lambda e, c=c, a=a: e.activation(out=sT[:, c, :], in_=tn[a], func=AF.Silu, bias=cvt[:, 8 + c:9 + c], scale=cvt[:, 4 + c:5 + c]),
                     reads=[r_tn[a], r_cv], writes=[r_sT])
            for co in range(4):
                b = 2 * (co % 2)
                for ci in range(4):
                    S.op('pe', lambda e, co=co, ci=ci, b=b: e.matmul(bank(b), lhsT=pwb[:, ci, 128 * co:128 * co + 128], rhs=sT[:, ci, :],
                                                                    start=(ci == 0), stop=(ci == 3)),
                         reads=[r_pwb, r_sT], writes=[rB[b]], acc=(ci > 0))
                S.op('act', lambda e, co=co, b=b, t=t: e.activation(out=convoutT[:, co, 512 * t:512 * t + 512], in_=bank(b), func=AF.Identity,
                                                                   bias=cvt[:, 12 + co:13 + co]),
                     reads=[rB[b], r_cv], writes=[r_convout])
        dump("convoutT", convoutT, [r_convout], [128, 4, TOK], BF16)
        S.barrier()
        cur[0] = xt_mark
        IDX = alloc([64], I32)
        GK = alloc([64], F32)
        mark2 = cur[0]

        wob = alloc([8, DM], BF16)
        r_wob = S.res()
        lnv = alloc([4, DM], F32)
        r_lnv = S.res()
        rwt = alloc([8, NE], F32)
        rbt = alloc([NE], F32)
        r_rw = S.res()
        xtk = [alloc([DM], F32) for _ in range(3)]
        r_xtk = [S.res() for _ in range(3)]
        LG = alloc([16, NE], F32)
        T8 = alloc([16, 8], F32)
        MKB = alloc([16, NE], BF16)
        r_lg = [S.res() for _ in range(16)]
        assert cur[0] <= attnT_mark, cur[0]
        cur[0] = conv_mark
        yb = [alloc([DM], F32) for _ in range(3)]
        r_yb = [S.res() for _ in range(3)]
        x1b = [alloc([DM], F32) for _ in range(3)]
        r_x1 = [S.res() for _ in range(3)]
        X1H = alloc([16, DM], BF16)
        r_x1h = [S.res() for _ in range(16)]
        x1T = [alloc([8, 128], F32) for _ in range(2)]
        r_x1T = [S.res() for _ in range(2)]
        lnt = [(alloc([12], F32), alloc([2], F32), alloc([1], F32), alloc([1], F32), S.res()) for _ in range(2)]
        DSTB = alloc([8, NE], F32)
        OH4 = alloc([8, 4, NE], F32)
        DK = alloc([32], F32)
        D4 = alloc([32], F32)
        S4 = alloc([8], F32)
        r_bt = S.res()
        r_x1d = [S.res() for _ in range(16)]

        S.dma('pool', lambda e: e.dma_start(out=wob, in_=w_out.rearrange("(k p) n -> p k n", p=128)), 'ldwo', writes=[r_wob])
        S.dma('sp', lambda e: e.dma_start(out=lnv, in_=lnvec.rearrange("p (a b) -> p a b", b=DM)), 'ldln', writes=[r_lnv])
        S.dma('sp', lambda e: e.dma_start(out=rwt, in_=rw.rearrange("(k p) n -> p k n", p=128)), 'ldrw', writes=[r_rw])
        S.dma('sp', lambda e: e.dma_start(out=rbt, in_=rbr), 'ldrw', writes=[r_rw], nowaw=True)

        catT = [convoutT[:, c, :] for c in range(4)] + [attnT[:, c, :] for c in range(4)]

        def fap(base, off, free):
            return bass.AP(base.tensor, base.offset + off, [list(base.ap[0])] + [list(x) for x in free])

        def ln_apply(src, rsrc, dsts, rdsts, gi, tmps, lnv, r_lnv, gb_eng='pool'):
            bst, mv, rs1, nmr, r_ln = tmps
            S.op('dve', lambda e: e.bn_stats(out=bst[:, 0:6], in_=src[:, 0:512]), reads=[rsrc], writes=[r_ln])
            S.op('dve', lambda e: e.bn_stats(out=bst[:, 6:12], in_=src[:, 512:1024]), reads=[rsrc], writes=[r_ln])
            S.op('dve', lambda e: e.bn_aggr(out=mv, in_=bst), reads=[r_ln], writes=[r_ln])
            S.op('dve', lambda e: e.tensor_scalar(out=rs1, in0=mv[:, 1:2], scalar1=LN_EPS, scalar2=None, op0=ALU.add), reads=[r_ln], writes=[r_ln])
            S.op('act', lambda e: e.activation(out=rs1, in_=rs1, func=AF.Sqrt), reads=[r_ln], writes=[r_ln])
            S.op('dve', lambda e: e.reciprocal(out=rs1, in_=rs1), reads=[r_ln], writes=[r_ln])
            S.op('dve', lambda e: e.tensor_scalar(out=nmr, in0=mv[:, 0:1], scalar1=rs1, scalar2=-1.0, op0=ALU.mult, op1=ALU.mult),
                 reads=[r_ln], writes=[r_ln])
            S.op('act', lambda e: e.activation(out=src, in_=src, func=AF.Identity, bias=nmr, scale=rs1), reads=[rsrc, r_ln], writes=[rsrc])
            S.op(gb_eng, lambda e: e.tensor_tensor(out=src, in0=src, in1=lnv[:, gi, :], op=ALU.mult), reads=[rsrc, r_lnv], writes=[rsrc])
            S.op(gb_eng, lambda e: e.tensor_tensor(out=dsts, in0=src, in1=lnv[:, gi + 1, :], op=ALU.add), reads=[rsrc, r_lnv], writes=[rdsts])

        def stage1(i):
            a = i % 3
            tsl = slice(128 * i, 128 * i + 128)
            S.dma('sp', lambda e, a=a, tsl=tsl: e.dma_start(out=xtk[a], in_=xtok[tsl, :]), f'ldxk{a}', writes=[r_xtk[a]])
            for h_ in range(2):
                b = 2 * (i % 2) + h_
                for fc in range(8):
                    S.op('pe', lambda e, fc=fc, b=b, h_=h_, tsl=tsl: e.matmul(bank(b), lhsT=catT[fc][:, tsl], rhs=wob[:, fc, 512 * h_:512 * h_ + 512],
                                                                             start=(fc == 0), stop=(fc == 7)),
                         reads=[r_convout, r_wob] + r_attnT, writes=[rB[b]], acc=(fc > 0))
                S.op('dve', lambda e, a=a, b=b, h_=h_: e.scalar_tensor_tensor(out=yb[a][:, 512 * h_:512 * h_ + 512], in0=xtk[a][:, 512 * h_:512 * h_ + 512],
                                                                             scalar=ALPHA, in1=bank(b), op0=ALU.mult, op1=ALU.add),
                     reads=[r_xtk[a], rB[b]], writes=[r_yb[a]])
            if i == 0:
                dump("y0", yb[0], [r_yb[0]], [128, DM])
            ln_apply(yb[a], r_yb[a], x1b[a], r_x1[a], 0, lnt[i % 2], lnv, r_lnv, gb_eng='dve')
            S.dma('sp', lambda e, a=a, tsl=tsl: e.dma_start(out=x1_d[tsl, :], in_=x1b[a]), f'stx1{a}', reads=[r_x1[a]], writes=[r_x1d[i]])
            S.op('act', lambda e, a=a, i=i: e.copy(out=X1H[:, i, :], in_=x1b[a]), reads=[r_x1[a]], writes=[r_x1h[i]])
            if i == 0:
                dump("x10", x1b[0], [r_x1[0]], [128, DM])

        def stage2(i):
            a = i % 3
            a2 = i % 2
            for kc in range(8):
                b = 4 + kc // 4
                S.op('pe', lambda e, kc=kc, b=b, a=a: e.transpose(bank(b)[:, 128 * (kc % 4):128 * (kc % 4) + 128], x1b[a][:, 128 * kc:128 * kc + 128], identf),
                     reads=[r_x1[a], r_const], writes=[rB[b]], acc=(kc % 4 > 0))
            for h_ in range(2):
                S.op('act' if h_ == 0 else 'dve',
                     (lambda e, a2=a2, h_=h_: e.copy(out=x1T[a2][:, 4 * h_:4 * h_ + 4, :], in_=bank(4 + h_).rearrange("p (k t) -> p k t", t=128))) if h_ == 0 else
                     (lambda e, a2=a2, h_=h_: e.tensor_copy(out=x1T[a2][:, 4 * h_:4 * h_ + 4, :], in_=bank(4 + h_).rearrange("p (k t) -> p k t", t=128))),
                     reads=[rB[4 + h_]], writes=[r_x1T[a2]])
            for kc in range(8):
                S.op('pe', lambda e, kc=kc, a2=a2: e.matmul(bank(6)[:, 0:NE], lhsT=x1T[a2][:, kc, :], rhs=rwt[:, kc, :], start=(kc == 0), stop=(kc == 7)),
                     reads=[r_x1T[a2], r_rw], writes=[rB[6]], acc=(kc > 0))
            S.op('dve', lambda e, i=i: e.tensor_tensor(out=LG[:, i, :], in0=bank(6)[:, 0:NE], in1=rbt, op=ALU.add), reads=[rB[6], r_rw], writes=[r_lg[i]])
            S.op('dve', lambda e, i=i: e.max(out=T8[:, i, :], in_=LG[:, i, :]), reads=[r_lg[i]], writes=[r_lg[i]])
            S.op('dve', lambda e, i=i: e.tensor_scalar(out=MKB[:, i, :], in0=LG[:, i, :], scalar1=T8[:, i, 3:4], scalar2=None, op0=ALU.is_ge),
                 reads=[r_lg[i]], writes=[r_lg[i]])

        def batch(bt):
            i0_ = 8 * bt
            for il in range(8):
                i = i0_ + il
                oc = bank(7)[:, 32 * il:32 * il + 32]
                S.op('pe', lambda e, i=i, oc=oc: e.matmul(oc, lhsT=trib, rhs=MKB[:, i, :], start=True, stop=(i == 0), skip_group_check=True),
                     reads=[r_lg[i], r_const], writes=[rB[7]], acc=(il > 0))
                for j in range(i):
                    S.op('pe', lambda e, j=j, i=i, oc=oc: e.matmul(oc, lhsT=onesb, rhs=MKB[:, j, :], start=False, stop=(j == i - 1), skip_group_check=True),
                         reads=[r_lg[j], r_const], writes=[rB[7]], acc=True)
            R = r_bt
            lgs = [r_lg[i0_ + il] for il in range(8)]
            S.op('dve', lambda e: e.tensor_tensor(out=DSTB, in0=bank(7)[:, 0:256].rearrange("p (t n) -> p t n", n=NE),
                                                  in1=fap(ecf, 0, [[0, 8], [1, NE]]), op=ALU.add),
                 reads=[rB[7], r_const], writes=[R])
            S.op('dve', lambda e: e.tensor_tensor(out=OH4, in0=fap(LG, i0_ * NE, [[NE, 8], [0, 4], [1, NE]]),
                                                  in1=fap(T8, i0_ * 8, [[8, 8], [1, 4], [0, NE]]), op=ALU.is_equal),
                 reads=lgs, writes=[R])
            S.op('dve', lambda e: e.tensor_tensor(out=OH4, in0=OH4, in1=fap(DSTB, 0, [[NE, 8], [0, 4], [1, NE]]), op=ALU.mult),
                 reads=[R], writes=[R])
            S.op('dve', lambda e: e.tensor_reduce(out=DK, in_=OH4.rearrange("p t k n -> p (t k) n"), axis=AX.X, op=ALU.add), reads=[R], writes=[R])
            S.op('dve', lambda e: e.tensor_copy(out=IDX[:, 32 * bt:32 * bt + 32], in_=DK), reads=[R], writes=[r_idx])
            S.op('dve', lambda e: e.tensor_tensor(out=D4.rearrange("p (t k) -> p t k", k=4), in0=fap(T8, i0_ * 8, [[8, 8], [1, 4]]),
                                                  in1=fap(T8, i0_ * 8, [[8, 8], [0, 4]]), op=ALU.subtract),
                 reads=lgs, writes=[R])
            S.op('act', lambda e: e.activation(out=D4, in_=D4, func=AF.Exp), reads=[R], writes=[R])
            S.op('dve', lambda e: e.tensor_reduce(out=S4, in_=D4.rearrange("p (t k) -> p t k", k=4), axis=AX.X, op=ALU.add), reads=[R], writes=[R])
            S.op('dve', lambda e: e.reciprocal(out=S4, in_=S4), reads=[R], writes=[R])
            S.op('dve', lambda e: e.tensor_tensor(out=GK[:, 32 * bt:32 * bt + 32].rearrange("p (t k) -> p t k", k=4),
                                                  in0=D4.rearrange("p (t k) -> p t k", k=4), in1=fap(S4, 0, [[1, 8], [0, 4]]), op=ALU.mult),
                 reads=[R], writes=[r_idx])
            for il in range(8):
                i = i0_ + il
                for k in range(4):
                    S.dma('pool', lambda e, k=k, i=i: e.indirect_dma_start(
                        out=xs_d, out_offset=bass.IndirectOffsetOnAxis(ap=IDX[:, 4 * i + k:4 * i + k + 1], axis=0),
                        in_=X1H[:, i, :], in_offset=None, bounds_check=bcreg(e), oob_is_err=False),
                        f'scat{bt}', reads=[r_x1h[i], r_idx], writes=[r_xs], nowaw='any')

        stage1(0)
        stage1(1)
        for i in range(16):
            if i + 2 < 16:
                stage1(i + 2)
            stage2(i)
            if i == 7:
                batch(0)
        batch(1)
        dump("LG", LG, r_lg, [128, 16, NE])
        dump("IDX", IDX, [r_idx], [128, 64], I32)
        dump("GK", GK, [r_idx], [128, 64])
        S.barrier()
        cur[0] = mark2
        if stop == 'router':
            final = []
            for k, v in S.cnt.items():
                if k.startswith('dbg'):
                    final.append(('d', k, v, 'sp'))
            S.wait_all('sp', final)
            S.emit()
            return nc, dbg_out

        NRING = 8
        Wr = [alloc([8, DM], BF16) for _ in range(NRING)]
        r_Wr = [S.res() for _ in range(NRING)]
        bgt = alloc([2 * NE * 8], F32)
        bu1 = alloc([NE * 8], F32)
        r_bg = S.res()
        xsb = [alloc([3, DM], BF16) for _ in range(2)]
        r_xsb = [S.res() for _ in range(2)]
        xsT = [alloc([8, CAP], BF16) for _ in range(2)]
        r_xsT = [S.res() for _ in range(2)]
        hT = alloc([8, CAP], BF16)
        r_hT = S.res()
        gc = [alloc([CAP], F32) for _ in range(2)]
        sgm = [alloc([CAP], F32) for _ in range(2)]
        u1 = [alloc([CAP], F32) for _ in range(2)]
        r_ew = [S.res() for _ in range(2)]
        yo = [alloc([DM], F32) for _ in range(3)]
        r_yo = [S.res() for _ in range(3)]
        r_ys = S.res("ys_scr")
        bdb = [alloc([DM], F32) for _ in range(2)]
        r_bdb = [S.res() for _ in range(2)]

        def load_bd(e_):
            s_ = e_ % 2
            S.dma('sp', lambda e, s_=s_, e_=e_: e.dma_start(out=bdb[s_], in_=bd_d[e_:e_ + 1, :].partition_broadcast(128)), f'ldbd{s_}', writes=[r_bdb[s_]])

        wsrc = [wg_d.rearrange("(e p) f -> p e f", p=128), wu_d.rearrange("(e p) f -> p e f", p=128),
                wd_d.rearrange("(e p) f -> p e f", p=128)]
        xs_v = xs_d.rearrange("(e s p) d -> p e s d", p=128, s=3)
        ycount = [0]

        def load_mat(n):
            if n >= 3 * NE:
                return
            e_, m, sl_ = n // 3, n % 3, n % NRING
            S.dma('pool', lambda e, m=m, sl_=sl_, e_=e_: e.dma_start(out=Wr[sl_].rearrange("p k n -> p (k n)"), in_=wsrc[m][:, e_], max_dma_last_dim=4096), f'ldW{sl_}', writes=[r_Wr[sl_]])

        def load_xs(e_):
            s_ = e_ % 2
            S.dma('sp', lambda e, s_=s_, e_=e_: e.dma_start(out=xsb[s_], in_=xs_v[:, e_]), f'ldxs{s_}', reads=[r_xs], writes=[r_xsb[s_]])

        def transposes(e_):
            s_ = e_ % 2
            for stl in range(3):
                b = stl % 2
                for kc in range(8):
                    S.op('pe', lambda e, b=b, kc=kc, stl=stl, s_=s_: e.transpose(bankb(b)[:, 128 * kc:128 * kc + 128], xsb[s_][:, stl, 128 * kc:128 * kc + 128], identb),
                         reads=[r_xsb[s_], r_const], writes=[rB[b]], acc=(kc > 0))
                S.op('act' if stl != 1 else 'dve',
                     (lambda e, b=b, stl=stl, s_=s_: e.copy(out=xsT[s_][:, :, 128 * stl:128 * stl + 128], in_=bankb(b).rearrange("p (k t) -> p k t", t=128))) if stl != 1 else
                     (lambda e, b=b, stl=stl, s_=s_: e.tensor_copy(out=xsT[s_][:, :, 128 * stl:128 * stl + 128], in_=bankb(b).rearrange("p (k t) -> p k t", t=128))),
                     reads=[rB[b]], writes=[r_xsT[s_]])

        for n in range(NRING):
            load_mat(n)
        S.dma('sp', lambda e: e.dma_start(out=bgt, in_=bgu), 'ldbg', writes=[r_bg])
        S.op('dve', lambda e: e.tensor_scalar(out=bu1, in0=bgt[:, NE * 8:2 * NE * 8], scalar1=1.0, scalar2=None, op0=ALU.add), reads=[r_bg], writes=[r_bg])
        load_xs(0)
        load_xs(1)
        load_bd(0)
        load_bd(1)
        transposes(0)
        for e_ in range(NE):
            s_ = e_ % 2
            Wg_, Wu_, Wd_ = Wr[(3 * e_) % NRING], Wr[(3 * e_ + 1) % NRING], Wr[(3 * e_ + 2) % NRING]
            rWg, rWu, rWd = r_Wr[(3 * e_) % NRING], r_Wr[(3 * e_ + 1) % NRING], r_Wr[(3 * e_ + 2) % NRING]
            if e_ == 0:
                dump("xsT0", xsT[0], [r_xsT[0]], [128, 8, CAP], BF16)
            for fc in range(8):
                a = fc % 2
                bG, bU = 2 + 2 * a, 3 + 2 * a
                for kc in range(8):
                    S.op('pe', lambda e, kc=kc, fc=fc, bG=bG, s_=s_, Wg_=Wg_: e.matmul(bank(bG)[:, 0:CAP], lhsT=Wg_[:, kc, 128 * fc:128 * fc + 128], rhs=xsT[s_][:, kc, :],
                                                                                      start=(kc == 0), stop=(kc == 7)),
                         reads=[rWg, r_xsT[s_]], writes=[rB[bG]], acc=(kc > 0))
                for kc in range(8):
                    S.op('pe', lambda e, kc=kc, fc=fc, bU=bU, s_=s_, Wu_=Wu_: e.matmul(bank(bU)[:, 0:CAP], lhsT=Wu_[:, kc, 128 * fc:128 * fc + 128], rhs=xsT[s_][:, kc, :],
                                                                                      start=(kc == 0), stop=(kc == 7)),
                         reads=[rWu, r_xsT[s_]], writes=[rB[bU]], acc=(kc > 0))
                bi = e_ * 8 + fc
                S.op('dve', lambda e, a=a, bG=bG, bi=bi: e.tensor_scalar(out=gc[a], in0=bank(bG)[:, 0:CAP], scalar1=bgt[:, bi:bi + 1], scalar2=7.0,
                                                                        op0=ALU.add, op1=ALU.min),
                     reads=[rB[bG], r_bg], writes=[r_ew[a]])
                S.op('act', lambda e, a=a: e.activation(out=sgm[a], in_=gc[a], func=AF.Silu, scale=1.702), reads=[r_ew[a]], writes=[r_ew[a]])
                S.op('dve', lambda e, a=a, bU=bU, bi=bi: e.tensor_scalar(out=u1[a], in0=bank(bU)[:, 0:CAP], scalar1=bu1[:, bi:bi + 1], scalar2=8.0,
                                                                        op0=ALU.add, op1=ALU.min),
                     reads=[rB[bU], r_bg], writes=[r_ew[a]])
                S.op('dve', lambda e, a=a, fc=fc: e.scalar_tensor_tensor(out=hT[:, fc, :], in0=u1[a], scalar=-6.0, in1=sgm[a], op0=ALU.max, op1=ALU.mult),
                     reads=[r_ew[a]], writes=[r_hT])
            load_mat(3 * e_ + NRING)
            load_mat(3 * e_ + 1 + NRING)
            if e_ == 0:
                dump("hT0", hT, [r_hT], [128, 8, CAP], BF16)
            if e_ + 1 < NE:
                transposes(e_ + 1)
            if e_ + 2 < NE:
                load_xs(e_ + 2)
            for stl in range(3):
                yi = ycount[0] % 3
                ycount[0] += 1
                for h_ in range(2):
                    b = 6 + h_
                    for fc in range(8):
                        S.op('pe', lambda e, fc=fc, b=b, h_=h_, stl=stl, Wd_=Wd_: e.matmul(bank(b), lhsT=hT[:, fc, 128 * stl:128 * stl + 128],
                                                                                          rhs=Wd_[:, fc, 512 * h_:512 * h_ + 512],
                                                                                          start=(fc == 0), stop=(fc == 7)),
                             reads=[r_hT, rWd], writes=[rB[b]], acc=(fc > 0))
                    S.op('dve', lambda e, b=b, h_=h_, yi=yi, s_=s_: e.scalar_tensor_tensor(out=yo[yi][:, 512 * h_:512 * h_ + 512], in0=bank(b), scalar=1.0 / 1.702,
                                                                                          in1=bdb[s_][:, 512 * h_:512 * h_ + 512], op0=ALU.mult, op1=ALU.add),
                         reads=[rB[b], r_bdb[s_]], writes=[r_yo[yi]])
                if e_ == 0 and stl == 0:
                    dump("yo0", yo[yi], [r_yo[yi]], [128, DM])
                row0 = e_ * CAP + 128 * stl
                S.dma('sp', lambda e, yi=yi, row0=row0: e.dma_start(out=ys_d[row0:row0 + 128, :], in_=yo[yi]), f'sty{yi}',
                      reads=[r_yo[yi]], writes=[r_ys], nowaw='any')
            load_mat(3 * e_ + 2 + NRING)
            if e_ + 2 < NE:
                load_bd(e_ + 2)
        S.barrier()
        cur[0] = mark2

        lnv2 = alloc([4, DM], F32)
        r_lnv2 = S.res()
        x1c = [alloc([DM], F32) for _ in range(3)]
        r_x1c = [S.res() for _ in range(3)]
        yk = [[alloc([DM], F32) for _ in range(4)] for _ in range(3)]
        r_yk = [[S.res() for _ in range(4)] for _ in range(3)]
        acc_ = [alloc([DM], F32) for _ in range(2)]
        r_acc = [S.res() for _ in range(2)]
        ob_ = [alloc([DM], F32) for _ in range(2)]
        r_ob = [S.res() for _ in range(2)]
        lnt2 = [(alloc([12], F32), alloc([2], F32), alloc([1], F32), alloc([1], F32), S.res()) for _ in range(2)]
        S.dma('sp', lambda e: e.dma_start(out=lnv2, in_=lnvec.rearrange("p (a b) -> p a b", b=DM)), 'ldln2', writes=[r_lnv2])
        out_toks = []

        def fetch(i):
            c3 = i % 3
            tsl = slice(128 * i, 128 * i + 128)
            S.dma('sp', lambda e, c3=c3, tsl=tsl: e.dma_start(out=x1c[c3], in_=x1_d[tsl, :]), f'ldx1{c3}', reads=[r_x1d[i]], writes=[r_x1c[c3]])
            for k in range(4):
                S.dma('pool', lambda e, c3=c3, k=k, i=i: e.indirect_dma_start(
                    out=yk[c3][k], out_offset=None, in_=ys_d,
                    in_offset=bass.IndirectOffsetOnAxis(ap=IDX[:, 4 * i + k:4 * i + k + 1], axis=0),
                    bounds_check=bcreg(e), oob_is_err=False),
                    f'gat{c3}{k}', reads=[r_ys, r_idx], writes=[r_yk[c3][k]])

        def accum(i):
            a = i % 2
            c3 = i % 3
            S.op('act', lambda e, a=a, c3=c3: e.activation(out=acc_[a], in_=x1c[c3], func=AF.Copy, scale=ALPHA), reads=[r_x1c[c3]], writes=[r_acc[a]])

        def accum2(i):
            a = i % 2
            c3 = i % 3
            for k in range(4):
                S.op('dve', lambda e, a=a, k=k, i=i, c3=c3: e.scalar_tensor_tensor(out=acc_[a], in0=yk[c3][k], scalar=GK[:, 4 * i + k:4 * i + k + 1], in1=acc_[a],
                                                                                  op0=ALU.mult, op1=ALU.add),
                     reads=[r_yk[c3][k], r_idx, r_acc[a]], writes=[r_acc[a]])

        def ln2_stats(i):
            a = i % 2
            src, rsrc = acc_[a], r_acc[a]
            bst, mv, rs1, nmr, r_ln = lnt2[i % 2]
            S.op('dve', lambda e: e.bn_stats(out=bst[:, 0:6], in_=src[:, 0:512]), reads=[rsrc], writes=[r_ln])
            S.op('dve', lambda e: e.bn_stats(out=bst[:, 6:12], in_=src[:, 512:1024]), reads=[rsrc], writes=[r_ln])
            S.op('dve', lambda e: e.bn_aggr(out=mv, in_=bst), reads=[r_ln], writes=[r_ln])
            S.op('dve', lambda e: e.tensor_scalar(out=rs1, in0=mv[:, 1:2], scalar1=LN_EPS, scalar2=None, op0=ALU.add), reads=[r_ln], writes=[r_ln])
            S.op('act', lambda e: e.activation(out=rs1, in_=rs1, func=AF.Sqrt), reads=[r_ln], writes=[r_ln])
            S.op('dve', lambda e: e.reciprocal(out=rs1, in_=rs1), reads=[r_ln], writes=[r_ln])
            S.op('dve', lambda e: e.tensor_scalar(out=nmr, in0=mv[:, 0:1], scalar1=rs1, scalar2=-1.0, op0=ALU.mult, op1=ALU.mult),
                 reads=[r_ln], writes=[r_ln])
            S.op('act', lambda e: e.activation(out=src, in_=src, func=AF.Identity, bias=nmr, scale=rs1), reads=[rsrc, r_ln], writes=[rsrc])

        def ln2_gb(i):
            a = i % 2
            tsl = slice(128 * i, 128 * i + 128)
            src, rsrc = acc_[a], r_acc[a]
            S.op('dve', lambda e: e.tensor_tensor(out=src, in0=src, in1=lnv2[:, 2, :], op=ALU.mult), reads=[rsrc, r_lnv2], writes=[rsrc])
            S.op('dve', lambda e: e.tensor_tensor(out=ob_[a], in0=src, in1=lnv2[:, 3, :], op=ALU.add), reads=[rsrc, r_lnv2], writes=[r_ob[a]])
            out_toks.append(S.dma('sp', lambda e, a=a, tsl=tsl: e.dma_start(out=out_d[tsl, :], in_=ob_[a]), f'sto{a}', reads=[r_ob[a]]))

        fetch(0)
        fetch(1)
        fetch(2)
        accum(0)
        accum2(0)
        for i in range(16):
            if i + 3 < 16:
                fetch(i + 3)
            if i + 1 < 16:
                accum(i + 1)
            ln2_stats(i)
            if i + 1 < 16:
                accum2(i + 1)
            ln2_gb(i)
        final = [t for t in out_toks]
        for k, v in S.cnt.items():
            if k.startswith('dbg'):
                final.append(('d', k, v, 'sp'))
        S.wait_all('sp', final)
        S.emit()
    return nc, dbg_out


def _inv_freq():
    e = 64
    try:
        import jax
        import jax.numpy as jnp
        with jax.default_device(jax.devices('cpu')[0]):
            v = 10000.0 ** (-jnp.arange(0, e, 2, dtype=jnp.float32) / e)
            return np.asarray(v, dtype=np.float32)
    except Exception:
        return (np.float32(10000.0) ** (-np.arange(0, e, 2, dtype=np.float32) / np.float32(e))).astype(np.float32)


def make_in_maps(x, positions, w_in, conv_w, conv_b, conv_ln_g, conv_ln_b, conv_pw_w, conv_pw_b, w_out, ln1_g, ln1_b,
                 router_w, router_b, exp_w_gate, exp_b_gate, exp_w_up, exp_b_up, exp_w_down, exp_b_down, ln2_g, ln2_b):
    f = lambda a: np.ascontiguousarray(np.asarray(a, dtype=np.float32))
    x = f(x)
    positions = np.asarray(positions).astype(np.int32)

    def p4(v):
        return f(v).reshape(4, 128).T
    cvec = np.ascontiguousarray(np.concatenate([p4(conv_b[0]), p4(conv_ln_g[0]), p4(conv_ln_b[0]), p4(conv_pw_b[0])], axis=1))
    convw = np.ascontiguousarray(f(conv_w[0]).T.reshape(4, 128, 31).transpose(1, 0, 2).reshape(128, 124))
    lnvec = np.ascontiguousarray(np.broadcast_to(np.concatenate([f(ln1_g[0]), f(ln1_b[0]), f(ln2_g[0]), f(ln2_b[0])])[None, :], (128, 4 * DM)))
    rbr = np.ascontiguousarray(np.broadcast_to(f(router_b[0])[None, :], (128, NE)))
    bg = f(exp_b_gate[0]).reshape(NE, 8, 128).transpose(2, 0, 1).reshape(128, NE * 8)
    bu = f(exp_b_up[0]).reshape(NE, 8, 128).transpose(2, 0, 1).reshape(128, NE * 8)
    bgu = np.ascontiguousarray(np.concatenate([bg, bu], axis=1))
    invf = _inv_freq()
    pidx = np.arange(128)

    def wtile(w):
        return np.ascontiguousarray(f(w).reshape(NE, 8, 128, DM).transpose(0, 2, 1, 3)).reshape(NE * 128, 8 * DM)
    shared = dict(
        w_in=f(w_in[0]), convw=convw, cvec=cvec, pw_w=f(conv_pw_w[0]), w_out=f(w_out[0]), lnvec=lnvec,
        rw=f(router_w[0]), rbr=rbr, wg=wtile(exp_w_gate[0]), wu=wtile(exp_w_up[0]),
        wd=wtile(exp_w_down[0]), bgu=bgu, bd=f(exp_b_down[0]),
        zsrc=np.zeros((256, DM), dtype=ml_dtypes.bfloat16),
    )
    maps = []
    for c in range(NCORES):
        b, qi = c // 4, c % 4
        t0 = TOK * qi
        xTc = np.zeros((DM, TT), np.float32)
        posc = np.zeros((1, TT), np.int32)
        if qi > 0:
            xTc[:, :TOK] = x[b, t0 - TOK:t0].T
            posc[0, :TOK] = positions[b, t0 - TOK:t0]
        xTc[:, TOK:] = x[b, t0:t0 + TOK].T
        posc[0, TOK:] = positions[b, t0:t0 + TOK]
        cstc = np.zeros((128, 4), np.float32)
        cstc[:, 0] = invf[pidx % 32]
        cstc[:, 1] = np.where((pidx % 64) < 32, -1.0, 1.0)
        cstc[:, 2] = 1.0 if qi > 0 else 0.0
        m = dict(shared)
        xTc = np.ascontiguousarray(xTc.reshape(8, 128, 8, 512).transpose(2, 1, 0, 3)).reshape(8, 128, 8 * 512)
        m.update(xT=xTc, xtok=np.ascontiguousarray(x[b, t0:t0 + TOK]), pos=posc, cst=cstc)
        maps.append(m)
    return maps


_NC_CACHE = {}


def kernel(**inputs):
    if 'nc' not in _NC_CACHE:
        _NC_CACHE['nc'] = build_program()[0]
    nc = _NC_CACHE['nc']
    in_maps = make_in_maps(**inputs)
    res = run_bass_kernel_spmd(nc, in_maps, core_ids=list(range(NCORES)))
    out = np.zeros((2, 8192, DM), np.float32)
    for c in range(NCORES):
        b, qi = c // 4, c % 4
        out[b, TOK * qi:TOK * qi + TOK] = np.asarray(res.results[c]["out"], dtype=np.float32)
    return out
```

```python
import math
import numpy as np
import ml_dtypes
from contextlib import ExitStack
import concourse.bass as bass
import concourse.mybir as mybir
from concourse.bass_utils import run_bass_kernel_spmd

F32 = mybir.dt.float32
BF16 = mybir.dt.bfloat16
I32 = mybir.dt.int32
ALU = mybir.AluOpType
AF = mybir.ActivationFunctionType
AX = mybir.AxisListType

NCORES = 8
TOK = 2048
TT = 4096
DM = 1024
NE = 32
CAP = 384
NSLOT = NE * CAP
ALPHA = float(2.0 ** 0.25)
LN_EPS = 1e-5
PI = math.pi
ENG = ['pe', 'act', 'dve', 'pool', 'sp']


class Res:
    __slots__ = ('name', 'w', 'r')

    def __init__(self, name):
        self.name = name
        self.w = None
        self.r = []


class Sched:
    def __init__(self, nc, stack):
        self.nc = nc
        self.stack = stack
        self.ops = {e: [] for e in ENG}
        self.cnt = {}
        self.seen = {e: {} for e in ENG}
        self.sems = {}
        self.clk = {}
        self.nres = 0

    def res(self, name=None):
        self.nres += 1
        return Res(name or f"r{self.nres}")

    def _tok_kv(self, tok):
        if tok[0] == 'd':
            return tok[1], tok[2]
        _, eng, idx = tok
        ops = self.ops[eng]
        for j in range(idx, len(ops)):
            s = ops[j][2]
            if s is not None and s[0] == eng:
                return eng, s[2]
        j = len(ops) - 1
        while ops[j][3] != 'c':
            j -= 1
        assert j >= idx
        v = self.cnt.get(eng, 0) + 1
        self.cnt[eng] = v
        ops[j][2] = [eng, 1, v]
        self.clk[(eng, v)] = ops[j][4]
        return eng, v

    def _need(self, eng, waits, tok, raw):
        if tok is None:
            return
        if tok[0] == 'c' and tok[1] == eng and not raw:
            return
        k, v = self._tok_kv(tok)
        if self.seen[eng].get(k, 0) >= v:
            return
        if waits.get(k, 0) < v:
            waits[k] = v

    def _commit_waits(self, eng, waits):
        seen = self.seen[eng]
        for k, v in waits.items():
            if seen.get(k, 0) < v:
                seen[k] = v
            c = self.clk.get((k, v))
            if c:
                for k2, v2 in c.items():
                    if seen.get(k2, 0) < v2:
                        seen[k2] = v2

    def op(self, eng, fn, reads=(), writes=(), acc=False):
        waits = {}
        for r in reads:
            self._need(eng, waits, r.w, True)
        if not acc:
            for w in writes:
                self._need(eng, waits, w.w, False)
                for t in w.r:
                    self._need(eng, waits, t, False)
        self._commit_waits(eng, waits)
        snap = dict(self.seen[eng])
        idx = len(self.ops[eng])
        self.ops[eng].append([waits, fn, None, 'c', snap])
        tok = ('c', eng, idx)
        for r in reads:
            r.r.append(tok)
        for w in writes:
            w.w = tok
            if not acc:
                w.r = []
        return tok

    def dma(self, eng, fn, sem, reads=(), writes=(), nowaw=False):
        waits = {}
        for r in reads:
            self._need(eng, waits, r.w, True)
        for w in writes:
            if not (nowaw == 'any' or (nowaw and w.w is not None and w.w[0] == 'd' and w.w[1] == sem)):
                self._need(eng, waits, w.w, True)
            for t in w.r:
                self._need(eng, waits, t, True)
        self._commit_waits(eng, waits)
        v = self.cnt.get(sem, 0) + 16
        self.cnt[sem] = v
        snap = dict(self.seen[eng])
        self.clk[(sem, v)] = snap
        self.ops[eng].append([waits, fn, [sem, 16, v], 'd', snap])
        tok = ('d', sem, v, eng)
        for r in reads:
            r.r.append(tok)
        for w in writes:
            w.w = tok
            w.r = []
        return tok

    def wait_all(self, eng, toks):
        waits = {}
        for t in toks:
            self._need(eng, waits, t, True)
        self._commit_waits(eng, waits)
        self.ops[eng].append([waits, None, None, 'w', None])

    def barrier(self, exclude=()):
        toks = []
        for e in ENG:
            ops = self.ops[e]
            for j in range(len(ops) - 1, -1, -1):
                if ops[j][3] == 'c':
                    toks.append(('c', e, j))
                    break
        for k, v in list(self.cnt.items()):
            if k not in ENG and not any(k.startswith(x) for x in exclude):
                toks.append(('d', k, v, None))
        for e in ENG:
            self.wait_all(e, toks)

    def emit(self):
        nc = self.nc
        for k in sorted(self.cnt.keys()):
            self.sems[k] = self.stack.enter_context(nc.semaphore("s_" + k))
        engs = {'pe': 'tensor', 'act': 'scalar', 'dve': 'vector', 'pool': 'gpsimd', 'sp': 'sync'}
        sems = self.sems
        with nc.Block() as block:
            for e in ENG:
                ops = self.ops[e]

                def body(engine, ops=ops):
                    for waits, fn, sig, kind, _ in ops:
                        for k, v in waits.items():
                            engine.wait_ge(sems[k], v)
                        if fn is None:
                            continue
                        ins = fn(engine)
                        if sig is not None:
                            ins.then_inc(sems[sig[0]], sig[1])
                getattr(block, engs[e])(body)


_REG = {}


def bcreg(e):
    if 'bc' not in _REG:
        _REG['bc'] = e.to_reg(NSLOT - 1)
    return _REG['bc']


def strided(ap, start, step, n):
    pat = [list(x) for x in ap.ap]
    es = pat[-1][0]
    return bass.AP(ap.tensor, ap.offset + start * es, pat[:-1] + [[step * es, n]])


def build_program(debug=None, stop=None):
    debug = debug or []
    nc = bass.Bass("TRN2", target_bir_lowering=False)
    _REG.clear()
    st = ExitStack()
    S = Sched(nc, st)

    def din(name, shape, dt=F32):
        return nc.dram_tensor(name, list(shape), dt, kind="ExternalInput").ap()

    xT = din("xT", [8, 128, 8 * 512])
    xtok = din("xtok", [TOK, DM])
    posd = din("pos", [1, TT], I32)
    w_in = din("w_in", [DM, 2560])
    convw = din("convw", [128, 4 * 31])
    cvec = din("cvec", [128, 16])
    pw_w = din("pw_w", [512, 512])
    w_out = din("w_out", [DM, DM])
    lnvec = din("lnvec", [128, 4 * DM])
    rw = din("rw", [DM, NE])
    rbr = din("rbr", [128, NE])
    if stop:
        wg_d = wu_d = wd_d = None
    else:
        wg_d = din("wg", [NE * 128, 8 * DM])
        wu_d = din("wu", [NE * 128, 8 * DM])
        wd_d = din("wd", [NE * 128, 8 * DM])
    bgu = din("bgu", [128, 2 * NE * 8])
    bd_d = din("bd", [NE, DM])
    cst = din("cst", [128, 4])
    zsrc = din("zsrc", [256, DM], BF16)
    out_d = nc.dram_tensor("out", [TOK, DM], F32, kind="ExternalOutput").ap()
    xs_d = nc.dram_tensor("xs_scr", [NSLOT, DM], BF16, kind="Internal").ap()
    ys_d = nc.dram_tensor("ys_scr", [NSLOT, DM], F32, kind="Internal").ap()
    x1_d = nc.dram_tensor("x1_scr", [TOK, DM], F32, kind="Internal").ap()
    dbg_out = {}

    ARENA = 207 * 1024
    with st:
        A = st.enter_context(nc.sbuf_tensor("arena", [128, ARENA // 2], BF16))
        PS = st.enter_context(nc.psum_tensor("PS", [128, 4096], F32))
        PSb = PS[:].bitcast(BF16)

        cur = [0]

        def alloc(shape, dt, name=None):
            n = int(np.prod(shape))
            nb = n * mybir.dt.size(dt)
            nb = (nb + 63) // 64 * 64
            off = cur[0]
            cur[0] += nb
            assert cur[0] <= ARENA, (name, cur[0])
            ap = A[:, off // 2: off // 2 + n * mybir.dt.size(dt) // 2]
            if dt != BF16:
                ap = ap.bitcast(dt)
            if len(shape) == 2:
                ap = ap.rearrange("p (a b) -> p a b", b=shape[1])
            elif len(shape) == 3:
                ap = ap.rearrange("p (a b c) -> p a b c", b=shape[1], c=shape[2])
            return ap

        def bank(b):
            return PS[:, 512 * b:512 * b + 512]

        def bankb(b):
            return PSb[:, 1024 * b:1024 * b + 1024]
        rB = [S.res(f"bank{b}") for b in range(8)]

        ndbg = [0]

        def dump(name, ap, res, shape, dt=F32):
            if name not in debug:
                return
            t = nc.dram_tensor("dbg_" + name, list(shape), dt, kind="ExternalOutput").ap()
            dbg_out[name] = t
            ndbg[0] += 1
            S.dma('sp', lambda e: e.dma_start(out=t, in_=ap), f'dbg{ndbg[0]}', reads=res)

        identf = alloc([128], F32)
        identb = alloc([128], BF16)
        onesb = alloc([128], BF16)
        onesf = alloc([128], F32)
        trib = alloc([128], BF16)
        M1 = alloc([512], BF16)
        M1f = alloc([512], BF16)
        M2f = alloc([512], BF16)
        M3 = alloc([4, 512], BF16)
        cstt = alloc([4], F32)
        ecf = alloc([NE], F32)
        r_const = S.res("const")
        r_idx = S.res("idx")
        mcf = alloc([128], F32)
        mpf_ = alloc([128], F32)
        mpff = alloc([128], F32)
        eci = alloc([NE], I32)
        xt_mark = cur[0]
        XT = alloc([8, 8, 512], BF16, "XT")
        rXT = [S.res(f"xt{i}") for i in range(8)]
        attnT_mark = cur[0]
        attnT = alloc([4, TOK], BF16, "attnT")
        r_attnT = [S.res(f"attnT{g}") for g in range(4)]
        persist_mark = cur[0]

        def load_xt(i):
            S.dma('pool', lambda e, i=i: e.dma_start(out=XT[:, i].rearrange("p k t -> p (k t)"), in_=xT[i], max_dma_last_dim=8192),
                  f'ldxt{i}', writes=[rXT[i]])
        r_cst = S.res("cst")
        S.dma('sp', lambda e: e.dma_start(out=cstt, in_=cst), 'ldc', writes=[r_cst])

        def build_consts():
            def aff(out, pattern, cm, op):
                S.op('pool', lambda e: e.memset(out, 1.0), writes=[r_const])
                S.op('pool', lambda e: e.affine_select(out=out, in_=out, pattern=pattern, compare_op=op, fill=0.0,
                                                       base=0, channel_multiplier=cm), reads=[r_const], writes=[r_const])
            aff(identf, [[-1, 128]], 1, ALU.is_equal)
            aff(mcf, [[1, 128]], -1, ALU.is_ge)
            aff(mpf_, [[-1, 128]], 1, ALU.is_ge)
            S.op('pool', lambda e: e.tensor_copy(out=identb, in_=identf), reads=[r_const], writes=[r_const])
            S.op('pool', lambda e: e.memset(onesb, 1.0), writes=[r_const])
            S.op('pool', lambda e: e.memset(onesf, 1.0), writes=[r_const])
            S.op('pool', lambda e: e.tensor_tensor(out=trib, in0=mcf, in1=identf, op=ALU.subtract), reads=[r_const], writes=[r_const])
            S.op('dve', lambda e: e.tensor_scalar(out=mpff, in0=mpf_, scalar1=cstt[:, 2:3], scalar2=None, op0=ALU.mult),
                 reads=[r_const, r_cst], writes=[r_const])
            for (M, parts) in ((M1, [mpf_, mcf, mpf_, mcf]), (M1f, [mpff, mcf, mpf_, mcf]), (M2f, [mpff, mcf, mpff, mcf])):
                for s_, src in enumerate(parts):
                    S.op('pool', lambda e, M=M, s_=s_, src=src: e.tensor_copy(out=M[:, 128 * s_:128 * s_ + 128], in_=src),
                         reads=[r_const], writes=[r_const])
            for tb in range(4):
                for r_ in range(8):
                    S.op('pool', lambda e, tb=tb, r_=r_: e.tensor_copy(out=M3[:, tb, 64 * r_:64 * r_ + 32], in_=mpff[:, 32 * tb:32 * tb + 32]),
                         reads=[r_const], writes=[r_const])
                    S.op('pool', lambda e, tb=tb, r_=r_: e.tensor_copy(out=M3[:, tb, 64 * r_ + 32:64 * r_ + 64], in_=mcf[:, 32 * tb:32 * tb + 32]),
                         reads=[r_const], writes=[r_const])
            S.op('pool', lambda e: e.iota(eci, pattern=[[CAP, NE]], base=0, channel_multiplier=0), writes=[r_const])
            S.op('pool', lambda e: e.tensor_copy(out=ecf, in_=eci), reads=[r_const], writes=[r_const])
        r_xs = S.res("xs_scr")

        cosT = alloc([TT], F32, "cos")
        sinT = alloc([TT], F32, "sin")
        r_tab = S.res("tab")
        wgt = alloc([8, 5, 128], BF16, "wgt")
        r_wgt = S.res("wgt")
        QT = alloc([TOK], BF16)
        KT = alloc([TT], BF16)
        VT = alloc([TT], BF16)
        r_QT = [S.res() for _ in range(4)]
        r_KT = [S.res() for _ in range(8)]
        r_VT = [S.res() for _ in range(8)]
        NKB = 69
        VTOK = alloc([NKB, 192], BF16)
        r_vtok = S.res("vtok")
        tmp_mark = cur[0]
        Ebuf = [alloc([512], BF16) for _ in range(3)]
        r_E = [S.res() for _ in range(3)]
        PTb = [alloc([512], BF16) for _ in range(3)]
        r_PT = [S.res() for _ in range(3)]
        lnb = [alloc([512], F32) for _ in range(2)]
        r_lnb = [S.res() for _ in range(2)]
        rdb = [alloc([512], F32) for _ in range(2)]
        r_rdb = [S.res() for _ in range(2)]
        alias_end = cur[0]
        rt = [alloc([512], F32) for _ in range(4)]
        r_rt = [S.res() for _ in range(4)]
        att_mark = cur[0]

        cur[0] = tmp_mark
        posi = alloc([512], I32)
        ang = alloc([512], F32)
        a2 = alloc([512], F32)
        kfi = alloc([512], I32)
        kff = alloc([512], F32)
        rr2 = [alloc([512], F32) for _ in range(2)]
        assert cur[0] <= alias_end, (cur[0], alias_end)
        r_pos = S.res("tabpos")
        r_tmp = S.res("tabtmp")
        r_rr = [S.res() for _ in range(2)]

        def load_pos(i):
            sl = slice(512 * i, 512 * i + 512)
            S.dma('sp', lambda e, sl=sl: e.dma_start(out=posi, in_=posd[:, sl].partition_broadcast(128)), 'ldpos', writes=[r_pos])

        def gen_tab(i):
            sl = slice(512 * i, 512 * i + 512)
            S.op('dve', lambda e: e.tensor_copy(out=ang, in_=posi), reads=[r_pos], writes=[r_tmp])
            if i + 1 < 8:
                load_pos(i + 1)
            S.op('dve', lambda e: e.tensor_scalar(out=ang, in0=ang, scalar1=cstt[:, 0:1], scalar2=None, op0=ALU.mult),
                 reads=[r_tmp, r_cst], writes=[r_tmp])
            for ti, (tab, shift, scl) in enumerate(((sinT, 0.0, cstt[:, 1:2]), (cosT, PI / 2, 1.0))):
                rr = rr2[ti]
                S.op('dve', lambda e, shift=shift: e.tensor_scalar(out=a2, in0=ang, scalar1=shift, scalar2=None, op0=ALU.add),
                     reads=[r_tmp], writes=[r_tmp])
                S.op('dve', lambda e: e.tensor_scalar(out=kfi, in0=a2, scalar1=1.0 / (2 * PI), scalar2=None, op0=ALU.mult),
                     reads=[r_tmp], writes=[r_tmp])
                S.op('dve', lambda e: e.tensor_copy(out=kff, in_=kfi), reads=[r_tmp], writes=[r_tmp])
                S.op('dve', lambda e, rr=rr: e.scalar_tensor_tensor(out=rr, in0=kff, scalar=-2 * PI, in1=a2, op0=ALU.mult, op1=ALU.add),
                     reads=[r_tmp], writes=[r_rr[ti]])
                S.op('dve', lambda e, rr=rr: e.tensor_scalar(out=rr, in0=rr, scalar1=-3.14159, scalar2=3.14159, op0=ALU.max, op1=ALU.min),
                     reads=[r_rr[ti]], writes=[r_rr[ti]])
                S.op('act', lambda e, tab=tab, sl=sl, scl=scl, rr=rr: e.activation(out=tab[:, sl], in_=rr, func=AF.Sin, scale=scl),
                     reads=[r_rr[ti], r_cst], writes=[r_tab])
        def load_wgt(g):
            q0, k0, v0 = 1024 + 128 * g, 1536 + 128 * g, 2048 + 128 * g
            for (slot, c0) in ((0, q0), (2, k0), (4, v0)):
                S.dma('pool', lambda e, slot=slot, c0=c0: e.dma_start(out=wgt[:, :, slot, :], in_=w_in_v[:, :, c0:c0 + 128]),
                      'ldwg', writes=[r_wgt], nowaw=True)
            for (slot, c0) in ((1, q0), (3, k0)):
                for hh in range(2):
                    for half in range(2):
                        d0 = 64 * hh + 32 * half
                        s0 = c0 + 64 * hh + 32 * (1 - half)
                        S.dma('pool', lambda e, slot=slot, d0=d0, s0=s0: e.dma_start(out=wgt[:, :, slot, d0:d0 + 32], in_=w_in_v[:, :, s0:s0 + 32]),
                              'ldwg', writes=[r_wgt], nowaw=True)

        w_in_v = w_in.rearrange("(k p) n -> p k n", p=128)
        load_xt(0)
        load_wgt(0)
        for i in range(1, 8):
            load_xt(i)
        load_pos(0)
        gen_tab(0)
        build_consts()
        S.op('pool', lambda e: e.memset(VTOK[:, :, 64:128], 1.0), writes=[r_vtok])
        cur[0] = att_mark

        w_in_v = w_in.rearrange("(k p) n -> p k n", p=128)

        def kcols(branch, r_, j):
            if branch == 1:
                return (2048 + 128 * j, 1)
            if branch == 2:
                return (2048 + 512 * j + r_, 4)
            return (2048 + 2048 * j + r_, 16)

        def vidx(branch, r_, j):
            if branch == 1:
                return j + 1
            if branch == 2:
                return 17 + r_ * 5 + (j + 1)
            return 37 + r_ * 2 + (j + 1)

        ucount = [0]
        for g in range(4):
            if g > 0:
                load_wgt(g)

            def proj(slot, b, i):
                for kc in range(8):
                    S.op('pe', lambda e, kc=kc: e.matmul(bank(b), lhsT=wgt[:, kc, slot, :], rhs=XT[:, i, kc, :],
                                                       start=(kc == 0), stop=(kc == 7)),
                         reads=[r_wgt, rXT[i]], writes=[rB[b]], acc=(kc > 0))

            pc = [0]

            def rope(dst, rdst, bA, bB, i):
                sl = slice(512 * i, 512 * i + 512)
                a = pc[0] % 2
                pc[0] += 1
                t1, t2 = rt[2 * a], rt[2 * a + 1]
                S.op('dve', lambda e: e.tensor_tensor(out=t1, in0=bank(bA), in1=cosT[:, sl], op=ALU.mult),
                     reads=[rB[bA], r_tab], writes=[r_rt[2 * a]])
                S.op('dve', lambda e: e.tensor_tensor(out=t2, in0=bank(bB), in1=sinT[:, sl], op=ALU.mult),
                     reads=[rB[bB], r_tab], writes=[r_rt[2 * a + 1]])
                S.op('pool', lambda e: e.tensor_tensor(out=dst, in0=t1, in1=t2, op=ALU.add),
                     reads=[r_rt[2 * a], r_rt[2 * a + 1]], writes=[rdst])

            for i in range(8):
                pb = 3 * (i % 2)
                if g == 0 and i + 1 < 8:
                    gen_tab(i + 1)
                proj(2, pb, i)
                proj(3, pb + 1, i)
                rope(KT[:, 512 * i:512 * i + 512], r_KT[i], pb, pb + 1, i)
                proj(4, pb + 2, i)
                S.op('act', lambda e, i=i, pb=pb: e.copy(out=VT[:, 512 * i:512 * i + 512], in_=bank(pb + 2)),
                     reads=[rB[pb + 2]], writes=[r_VT[i]])
                if i >= 4:
                    proj(0, 6, i)
                    proj(1, 7, i)
                    rope(QT[:, 512 * (i - 4):512 * (i - 4) + 512], r_QT[i - 4], 6, 7, i)
            if g == 0:
                dump("KT0", KT, r_KT, [128, TT], BF16)
                dump("QT0", QT, r_QT, [128, TOK], BF16)
                dump("VT0", VT, r_VT, [128, TT], BF16)

            kbs = []
            for j in range(-1, 16):
                kbs.append((1, 0, j))
            for r_ in range(4):
                for j in range(-1, 4):
                    kbs.append((2, r_, j))
            for r_ in range(16):
                for j in range(-1, 1):
                    kbs.append((3, r_, j))
            assert len(kbs) == NKB
            for c0 in range(0, NKB, 8):
                chunk = kbs[c0:c0 + 8]
                b = 6 + (c0 // 8) % 2
                for s_, (br, r_, j) in enumerate(chunk):
                    stt, stp = kcols(br, r_, j)
                    assert vidx(br, r_, j) == c0 + s_
                    S.op('pe', lambda e, b=b, s_=s_, stt=stt, stp=stp: e.transpose(bankb(b)[:, 128 * s_:128 * s_ + 128], strided(VT, stt, stp, 128), identb),
                         reads=r_VT + [r_const], writes=[rB[b]], acc=(s_ > 0))
                n = len(chunk)
                src = bankb(b)[:, 0:128 * n].rearrange("p (n f) -> p n f", f=128)
                S.op('act', lambda e, c0=c0, n=n, src=src: e.copy(out=VTOK[:, c0:c0 + n, 0:64], in_=src[:, :, 0:64]),
                     reads=[rB[b]], writes=[r_vtok])
                S.op('dve', lambda e, c0=c0, n=n, src=src: e.tensor_copy(out=VTOK[:, c0:c0 + n, 128:192], in_=src[:, :, 64:128]),
                     reads=[rB[b]], writes=[r_vtok])

            units = []
            for tb in range(4):
                for hh in range(2):
                    hs = slice(64 * hh, 64 * hh + 64)
                    KTh, QTh = KT[hs, :], QT[hs, :]
                    ob = 4 + (tb * 2 + hh) % 2
                    O = bank(ob)
                    ulist = []
                    for half in range(2):
                        smm, pv = [], []
                        for s_ in range(2):
                            qb = 4 * tb + 2 * half + s_
                            for w_, j in enumerate((qb - 1, qb)):
                                ks, kp = kcols(1, 0, j)
                                col = 256 * s_ + 128 * w_
                                smm.append((strided(KTh, ks, kp, 128), QTh[:, 128 * qb:128 * qb + 128], col, 128))
                                pv.append((vidx(1, 0, j), col, 128, O[:, 128 * (qb - 4 * tb):128 * (qb - 4 * tb) + 128]))
                        mask = M1f if (tb == 0 and half == 0) else M1
                        ulist.append((smm, mask, pv))
                    for half in range(2):
                        smm, pv = [], []
                        for s_ in range(2):
                            r_ = 2 * half + s_
                            for w_, j in enumerate((tb - 1, tb)):
                                ks, kp = kcols(2, r_, j)
                                col = 256 * s_ + 128 * w_
                                smm.append((strided(KTh, ks, kp, 128), strided(QTh, 512 * tb + r_, 4, 128), col, 128))
                                pv.append((vidx(2, r_, j), col, 128, strided(O, r_, 4, 128)))
                        mask = M2f if tb == 0 else M1
                        ulist.append((smm, mask, pv))
                    for half in range(2):
                        smm, pv = [], []
                        for s_ in range(8):
                            r_ = 8 * half + s_
                            for w_, j in enumerate((-1, 0)):
                                ks, kp = kcols(3, r_, j)
                                col = 64 * s_ + 32 * w_
                                smm.append((strided(KTh, ks, kp, 128), strided(QTh, 512 * tb + r_, 16, 32), col, 32))
                                pv.append((vidx(3, r_, j), col, 32, strided(O, r_, 16, 32)))
                        ulist.append((smm, M3[:, tb, :], pv))
                    for ui, u in enumerate(ulist):
                        units.append((tb, hh, ob, ui, u))

            def emit_S(un, sb, eb):
                tb, hh, ob, ui, (smm, mask, pv) = un
                for n_, (lh, rh, col, N) in enumerate(smm):
                    S.op('pe', lambda e, lh=lh, rh=rh, col=col, N=N: e.matmul(bank(sb)[:, col:col + N], lhsT=lh, rhs=rh, start=True, stop=True,
                                                                              skip_group_check=True),
                         reads=r_KT + r_QT, writes=[rB[sb]], acc=(n_ > 0))
                S.op('act', lambda e: e.activation(out=Ebuf[eb], in_=bank(sb), func=AF.Exp, scale=0.125),
                     reads=[rB[sb]], writes=[r_E[eb]])
                eng = 'dve' if (ucount[0] % 3 != 2) else 'pool'
                S.op(eng, lambda e: e.tensor_tensor(out=PTb[eb], in0=Ebuf[eb], in1=mask, op=ALU.mult),
                     reads=[r_E[eb], r_const], writes=[r_PT[eb]])

            def emit_PV(un, eb):
                tb, hh, ob, ui, (smm, mask, pv) = un
                for n_, (vi, col, N, oap) in enumerate(pv):
                    first = (ui == 0 and n_ == 0)
                    last = (ui == 5 and n_ == len(pv) - 1)
                    lh = VTOK[:, vi, 0:128] if hh == 0 else VTOK[:, vi, 64:192]
                    S.op('pe', lambda e, lh=lh, col=col, N=N, oap=oap, first=first, last=last: e.matmul(
                        oap, lhsT=lh, rhs=PTb[eb][:, col:col + N], start=first, stop=last, skip_group_check=True),
                        reads=[r_vtok, r_PT[eb]], writes=[rB[ob]], acc=(not first))
                if ui == 5:
                    num = slice(0, 64) if hh == 0 else slice(64, 128)
                    den = slice(64, 128) if hh == 0 else slice(0, 64)
                    a = (tb * 2 + hh) % 2
                    dsta = attnT[num, g, 512 * tb:512 * tb + 512]
                    S.op('act', lambda e: e.activation(out=lnb[a][den, :], in_=bank(ob)[den, :], func=AF.Ln),
                         reads=[rB[ob]], writes=[r_lnb[a]])
                    S.op('act', lambda e: e.activation(out=rdb[a][den, :], in_=lnb[a][den, :], func=AF.Exp, scale=-1.0),
                         reads=[r_lnb[a]], writes=[r_rdb[a]])
                    S.op('dve', lambda e: e.tensor_tensor(out=dsta, in0=bank(ob)[num, :], in1=rdb[a][den, :], op=ALU.mult),
                         reads=[rB[ob], r_rdb[a]], writes=[r_attnT[g]])

            nun = len(units)
            for u in range(nun + 1):
                if u < nun:
                    emit_S(units[u], u % 4, u % 3)
                    ucount[0] += 1
                if u >= 1:
                    emit_PV(units[u - 1], (u - 1) % 3)
        dump("attnT", attnT, r_attnT, [128, 4, TOK], BF16)
        S.barrier()
        cur[0] = persist_mark

        convoutT = alloc([4, TOK], BF16, "convoutT")
        r_convout = S.res("convout")
        conv_mark = cur[0]
        uT = alloc([4, TOK + 32], BF16)
        r_uT = [S.res() for _ in range(5)]
        diag = alloc([124, 128], BF16)
        r_diag = S.res()
        cwt = alloc([124], F32)
        cvt = alloc([16], F32)
        r_cv = S.res()
        pwb = alloc([4, 512], BF16)
        r_pwb = S.res()
        r_cT = S.res()
        r_csq = S.res()
        sg = [alloc([512], F32) for _ in range(2)]
        r_sg = [S.res() for _ in range(2)]
        mean = alloc([512], F32)
        msq = alloc([512], F32)
        var = alloc([512], F32)
        rstd = alloc([512], F32)
        r_st = S.res()
        tn = [alloc([512], F32) for _ in range(2)]
        r_tn = [S.res() for _ in range(2)]
        sT = alloc([4, 512], BF16)
        r_sT = S.res()
        wag_mark = cur[0]
        wag = alloc([8, 1024], BF16)
        r_wag = S.res()
        cur[0] = wag_mark
        cT = alloc([4, 512], F32)
        csq = alloc([4, 512], F32)

        for h_ in range(2):
            S.dma('pool', lambda e, h_=h_: e.dma_start(out=wag[:, :, 512 * h_:512 * h_ + 512], in_=w_in_v[:, :, 512 * h_:512 * h_ + 512]),
                  'ldwag', writes=[r_wag], nowaw=True)
        S.dma('sp', lambda e: e.dma_start(out=cwt, in_=convw), 'ldcw', writes=[r_cv])
        S.dma('sp', lambda e: e.dma_start(out=cvt, in_=cvec), 'ldcv', writes=[r_cv], nowaw=True)
        S.dma('pool', lambda e: e.dma_start(out=pwb, in_=pw_w.rearrange("(k p) n -> p k n", p=128)), 'ldpw', writes=[r_pwb])
        for c in range(4):
            for j in range(31):
                n_ = c * 31 + j
                S.op('dve', lambda e, n_=n_: e.tensor_scalar(out=diag[:, n_, :], in0=identf, scalar1=cwt[:, n_:n_ + 1], scalar2=None, op0=ALU.mult),
                     reads=[r_cv, r_const], writes=[r_diag])

        def glu(xcols, ucols, ncol, ru, xi):
            for c in range(4):
                bA, bG = 2 * (c % 2), 2 * (c % 2) + 1
                for kc in range(8):
                    S.op('pe', lambda e, kc=kc, c=c, bA=bA: e.matmul(bank(bA)[:, 0:ncol], lhsT=wag[:, kc, 128 * c:128 * c + 128], rhs=XT[:, xi, kc, xcols],
                                                                    start=(kc == 0), stop=(kc == 7)),
                         reads=[r_wag, rXT[xi]], writes=[rB[bA]], acc=(kc > 0))
                for kc in range(8):
                    S.op('pe', lambda e, kc=kc, c=c, bG=bG: e.matmul(bank(bG)[:, 0:ncol], lhsT=wag[:, kc, 512 + 128 * c:512 + 128 * c + 128], rhs=XT[:, xi, kc, xcols],
                                                                    start=(kc == 0), stop=(kc == 7)),
                         reads=[r_wag, rXT[xi]], writes=[rB[bG]], acc=(kc > 0))
                a = c % 2
                S.op('act', lambda e, a=a, bG=bG: e.activation(out=sg[a][:, 0:ncol], in_=bank(bG)[:, 0:ncol], func=AF.Sigmoid),
                     reads=[rB[bG]], writes=[r_sg[a]])
                S.op('dve', lambda e, a=a, bA=bA, c=c: e.tensor_tensor(out=uT[:, c, ucols], in0=bank(bA)[:, 0:ncol], in1=sg[a][:, 0:ncol], op=ALU.mult),
                     reads=[rB[bA], r_sg[a]], writes=[ru])
        glu(slice(480, 512), slice(0, 32), 32, r_uT[0], 3)
        for t in range(4):
            glu(slice(0, 512), slice(32 + 512 * t, 32 + 512 * t + 512), 512, r_uT[t + 1], 4 + t)
        dump("uT", uT, r_uT, [128, 4, TOK + 32], BF16)
        xs_z = xs_d.rearrange("(n r) d -> n r d", r=256)
        for n_ in range(NSLOT // 256):
            S.dma('sp', lambda e, n_=n_: e.dma_start(out=xs_z[n_], in_=zsrc), 'zxs', writes=[r_xs], nowaw=True)

        for t in range(4):
            ureads = [r_uT[t], r_uT[t + 1]] if t > 0 else [r_uT[0], r_uT[1]]
            for c in range(4):
                b = 4 + c % 2
                for j in range(31):
                    S.op('pe', lambda e, c=c, j=j, b=b, t=t: e.matmul(bank(b), lhsT=diag[:, c * 31 + j, :], rhs=uT[:, c, 512 * t + 2 + j:512 * t + 2 + j + 512],
                                                                     start=(j == 0), stop=(j == 30)),
                         reads=[r_diag] + ureads, writes=[rB[b]], acc=(j > 0))
                S.op('act', lambda e, c=c, b=b: e.activation(out=cT[:, c, :], in_=bank(b), func=AF.Identity, bias=cvt[:, c:c + 1]),
                     reads=[rB[b], r_cv], writes=[r_cT])
                S.op('act', lambda e, c=c: e.activation(out=csq[:, c, :], in_=cT[:, c, :], func=AF.Square),
                     reads=[r_cT], writes=[r_csq])
            if t == 0:
                dump("cT0", cT, [r_cT], [128, 4, 512])
            for c in range(4):
                S.op('pe', lambda e, c=c: e.matmul(bank(6), lhsT=onesf, rhs=cT[:, c, :], start=(c == 0), stop=(c == 3)),
                     reads=[r_cT, r_const], writes=[rB[6]], acc=(c > 0))
            for c in range(4):
                S.op('pe', lambda e, c=c: e.matmul(bank(7), lhsT=onesf, rhs=csq[:, c, :], start=(c == 0), stop=(c == 3)),
                     reads=[r_csq, r_const], writes=[rB[7]], acc=(c > 0))
            S.op('act', lambda e: e.activation(out=mean, in_=bank(6), func=AF.Copy, scale=1.0 / 512), reads=[rB[6]], writes=[r_st])
            S.op('dve', lambda e: e.tensor_tensor(out=msq, in0=mean, in1=mean, op=ALU.mult), reads=[r_st], writes=[r_st])
            S.op('dve', lambda e: e.scalar_tensor_tensor(out=var, in0=bank(7), scalar=1.0 / 512, in1=msq, op0=ALU.mult, op1=ALU.subtract),
                 reads=[rB[7], r_st], writes=[r_st])
            S.op('dve', lambda e: e.tensor_scalar(out=var, in0=var, scalar1=LN_EPS, scalar2=None, op0=ALU.add), reads=[r_st], writes=[r_st])
            S.op('act', lambda e: e.activation(out=var, in_=var, func=AF.Ln), reads=[r_st], writes=[r_st])
            S.op('act', lambda e: e.activation(out=rstd, in_=var, func=AF.Exp, scale=-0.5), reads=[r_st], writes=[r_st])
            for c in range(4):
                a = c % 2
                S.op('dve', lambda e, c=c, a=a: e.tensor_tensor(out=tn[a], in0=cT[:, c, :], in1=mean, op=ALU.subtract),
                     reads=[r_cT, r_st], writes=[r_tn[a]])
                S.op('pool', lambda e, a=a: e.tensor_tensor(out=tn[a], in0=tn[a], in1=rstd, op=ALU.mult),
                     reads=[r_tn[a], r_st], writes=[r_tn[a]])
                S.op('act', lambda e, c=c, a=a: e.activation(out=sT[:, c, :], in_=tn[a], func=AF.Silu, bias=cvt[:, 8 + c:9 + c], scale=cvt[:, 4 + c:5 + c]),
                     reads=[r_tn[a], r_cv], writes=[r_sT])
            for co in range(4):
                b = 2 * (co % 2)
                for ci in range(4):
                    S.op('pe', lambda e, co=co, ci=ci, b=b: e.matmul(bank(b), lhsT=pwb[:, ci, 128 * co:128 * co + 128], rhs=sT[:, ci, :],
                                                                    start=(ci == 0), stop=(ci == 3)),
                         reads=[r_pwb, r_sT], writes=[rB[b]], acc=(ci > 0))
                S.op('act', lambda e, co=co, b=b, t=t: e.activation(out=convoutT[:, co, 512 * t:512 * t + 512], in_=bank(b), func=AF.Identity,
                                                                   bias=cvt[:, 12 + co:13 + co]),
                     reads=[rB[b], r_cv], writes=[r_convout])
        dump("convoutT", convoutT, [r_convout], [128, 4, TOK], BF16)
        S.barrier()
        cur[0] = xt_mark
        IDX = alloc([64], I32)
        GK = alloc([64], F32)
        mark2 = cur[0]

        wob = alloc([8, DM], BF16)
        r_wob = S.res()
        lnv = alloc([4, DM], F32)
        r_lnv = S.res()
        rwt = alloc([8, NE], F32)
        rbt = alloc([NE], F32)
        r_rw = S.res()
        xtk = [alloc([DM], F32) for _ in range(3)]
        r_xtk = [S.res() for _ in range(3)]
        LG = alloc([16, NE], F32)
        T8 = alloc([16, 8], F32)
        MKB = alloc([16, NE], BF16)
        r_lg = [S.res() for _ in range(16)]
        assert cur[0] <= attnT_mark, cur[0]
        cur[0] = conv_mark
        yb = [alloc([DM], F32) for _ in range(3)]
        r_yb = [S.res() for _ in range(3)]
        x1b = [alloc([DM], F32) for _ in range(3)]
        r_x1 = [S.res() for _ in range(3)]
        X1H = alloc([16, DM], BF16)
        r_x1h = [S.res() for _ in range(16)]
        x1T = [alloc([8, 128], F32) for _ in range(2)]
        r_x1T = [S.res() for _ in range(2)]
        lnt = [(alloc([12], F32), alloc([2], F32), alloc([1], F32), alloc([1], F32), S.res()) for _ in range(2)]
        DSTB = alloc([8, NE], F32)
        OH4 = alloc([8, 4, NE], F32)
        DK = alloc([32], F32)
        D4 = alloc([32], F32)
        S4 = alloc([8], F32)
        r_bt = S.res()
        r_x1d = [S.res() for _ in range(16)]

        S.dma('pool', lambda e: e.dma_start(out=wob, in_=w_out.rearrange("(k p) n -> p k n", p=128)), 'ldwo', writes=[r_wob])
        S.dma('sp', lambda e: e.dma_start(out=lnv, in_=lnvec.rearrange("p (a b) -> p a b", b=DM)), 'ldln', writes=[r_lnv])
        S.dma('sp', lambda e: e.dma_start(out=rwt, in_=rw.rearrange("(k p) n -> p k n", p=128)), 'ldrw', writes=[r_rw])
        S.dma('sp', lambda e: e.dma_start(out=rbt, in_=rbr), 'ldrw', writes=[r_rw], nowaw=True)

        catT = [convoutT[:, c, :] for c in range(4)] + [attnT[:, c, :] for c in range(4)]

        def fap(base, off, free):
            return bass.AP(base.tensor, base.offset + off, [list(base.ap[0])] + [list(x) for x in free])

        def ln_apply(src, rsrc, dsts, rdsts, gi, tmps, lnv, r_lnv, gb_eng='pool'):
            bst, mv, rs1, nmr, r_ln = tmps
            S.op('dve', lambda e: e.bn_stats(out=bst[:, 0:6], in_=src[:, 0:512]), reads=[rsrc], writes=[r_ln])
            S.op('dve', lambda e: e.bn_stats(out=bst[:, 6:12], in_=src[:, 512:1024]), reads=[rsrc], writes=[r_ln])
            S.op('dve', lambda e: e.bn_aggr(out=mv, in_=bst), reads=[r_ln], writes=[r_ln])
            S.op('dve', lambda e: e.tensor_scalar(out=rs1, in0=mv[:, 1:2], scalar1=LN_EPS, scalar2=None, op0=ALU.add), reads=[r_ln], writes=[r_ln])
            S.op('act', lambda e: e.activation(out=rs1, in_=rs1, func=AF.Sqrt), reads=[r_ln], writes=[r_ln])
            S.op('dve', lambda e: e.reciprocal(out=rs1, in_=rs1), reads=[r_ln], writes=[r_ln])
            S.op('dve', lambda e: e.tensor_scalar(out=nmr, in0=mv[:, 0:1], scalar1=rs1, scalar2=-1.0, op0=ALU.mult, op1=ALU.mult),
                 reads=[r_ln], writes=[r_ln])
            S.op('act', lambda e: e.activation(out=src, in_=src, func=AF.Identity, bias=nmr, scale=rs1), reads=[rsrc, r_ln], writes=[rsrc])
            S.op(gb_eng, lambda e: e.tensor_tensor(out=src, in0=src, in1=lnv[:, gi, :], op=ALU.mult), reads=[rsrc, r_lnv], writes=[rsrc])
            S.op(gb_eng, lambda e: e.tensor_tensor(out=dsts, in0=src, in1=lnv[:, gi + 1, :], op=ALU.add), reads=[rsrc, r_lnv], writes=[rdsts])

        def stage1(i):
            a = i % 3
            tsl = slice(128 * i, 128 * i + 128)
            S.dma('sp', lambda e, a=a, tsl=tsl: e.dma_start(out=xtk[a], in_=xtok[tsl, :]), f'ldxk{a}', writes=[r_xtk[a]])
            for h_ in range(2):
                b = 2 * (i % 2) + h_
                for fc in range(8):
                    S.op('pe', lambda e, fc=fc, b=b, h_=h_, tsl=tsl: e.matmul(bank(b), lhsT=catT[fc][:, tsl], rhs=wob[:, fc, 512 * h_:512 * h_ + 512],
                                                                             start=(fc == 0), stop=(fc == 7)),
                         reads=[r_convout, r_wob] + r_attnT, writes=[rB[b]], acc=(fc > 0))
                S.op('dve', lambda e, a=a, b=b, h_=h_: e.scalar_tensor_tensor(out=yb[a][:, 512 * h_:512 * h_ + 512], in0=xtk[a][:, 512 * h_:512 * h_ + 512],
                                                                             scalar=ALPHA, in1=bank(b), op0=ALU.mult, op1=ALU.add),
                     reads=[r_xtk[a], rB[b]], writes=[r_yb[a]])
            if i == 0:
                dump("y0", yb[0], [r_yb[0]], [128, DM])
            ln_apply(yb[a], r_yb[a], x1b[a], r_x1[a], 0, lnt[i % 2], lnv, r_lnv, gb_eng='dve')
            S.dma('sp', lambda e, a=a, tsl=tsl: e.dma_start(out=x1_d[tsl, :], in_=x1b[a]), f'stx1{a}', reads=[r_x1[a]], writes=[r_x1d[i]])
            S.op('act', lambda e, a=a, i=i: e.copy(out=X1H[:, i, :], in_=x1b[a]), reads=[r_x1[a]], writes=[r_x1h[i]])
            if i == 0:
                dump("x10", x1b[0], [r_x1[0]], [128, DM])

        def stage2(i):
            a = i % 3
            a2 = i % 2
            for kc in range(8):
                b = 4 + kc // 4
                S.op('pe', lambda e, kc=kc, b=b, a=a: e.transpose(bank(b)[:, 128 * (kc % 4):128 * (kc % 4) + 128], x1b[a][:, 128 * kc:128 * kc + 128], identf),
                     reads=[r_x1[a], r_const], writes=[rB[b]], acc=(kc % 4 > 0))
            for h_ in range(2):
                S.op('act' if h_ == 0 else 'dve',
                     (lambda e, a2=a2, h_=h_: e.copy(out=x1T[a2][:, 4 * h_:4 * h_ + 4, :], in_=bank(4 + h_).rearrange("p (k t) -> p k t", t=128))) if h_ == 0 else
                     (lambda e, a2=a2, h_=h_: e.tensor_copy(out=x1T[a2][:, 4 * h_:4 * h_ + 4, :], in_=bank(4 + h_).rearrange("p (k t) -> p k t", t=128))),
                     reads=[rB[4 + h_]], writes=[r_x1T[a2]])
            for kc in range(8):
                S.op('pe', lambda e, kc=kc, a2=a2: e.matmul(bank(6)[:, 0:NE], lhsT=x1T[a2][:, kc, :], rhs=rwt[:, kc, :], start=(kc == 0), stop=(kc == 7)),
                     reads=[r_x1T[a2], r_rw], writes=[rB[6]], acc=(kc > 0))
            S.op('dve', lambda e, i=i: e.tensor_tensor(out=LG[:, i, :], in0=bank(6)[:, 0:NE], in1=rbt, op=ALU.add), reads=[rB[6], r_rw], writes=[r_lg[i]])
            S.op('dve', lambda e, i=i: e.max(out=T8[:, i, :], in_=LG[:, i, :]), reads=[r_lg[i]], writes=[r_lg[i]])
            S.op('dve', lambda e, i=i: e.tensor_scalar(out=MKB[:, i, :], in0=LG[:, i, :], scalar1=T8[:, i, 3:4], scalar2=None, op0=ALU.is_ge),
                 reads=[r_lg[i]], writes=[r_lg[i]])

        def batch(bt):
            i0_ = 8 * bt
            for il in range(8):
                i = i0_ + il
                oc = bank(7)[:, 32 * il:32 * il + 32]
                S.op('pe', lambda e, i=i, oc=oc: e.matmul(oc, lhsT=trib, rhs=MKB[:, i, :], start=True, stop=(i == 0), skip_group_check=True),
                     reads=[r_lg[i], r_const], writes=[rB[7]], acc=(il > 0))
                for j in range(i):
                    S.op('pe', lambda e, j=j, i=i, oc=oc: e.matmul(oc, lhsT=onesb, rhs=MKB[:, j, :], start=False, stop=(j == i - 1), skip_group_check=True),
                         reads=[r_lg[j], r_const], writes=[rB[7]], acc=True)
            R = r_bt
            lgs = [r_lg[i0_ + il] for il in range(8)]
            S.op('dve', lambda e: e.tensor_tensor(out=DSTB, in0=bank(7)[:, 0:256].rearrange("p (t n) -> p t n", n=NE),
                                                  in1=fap(ecf, 0, [[0, 8], [1, NE]]), op=ALU.add),
                 reads=[rB[7], r_const], writes=[R])
            S.op('dve', lambda e: e.tensor_tensor(out=OH4, in0=fap(LG, i0_ * NE, [[NE, 8], [0, 4], [1, NE]]),
                                                  in1=fap(T8, i0_ * 8, [[8, 8], [1, 4], [0, NE]]), op=ALU.is_equal),
                 reads=lgs, writes=[R])
            S.op('dve', lambda e: e.tensor_tensor(out=OH4, in0=OH4, in1=fap(DSTB, 0, [[NE, 8], [0, 4], [1, NE]]), op=ALU.mult),
                 reads=[R], writes=[R])
            S.op('dve', lambda e: e.tensor_reduce(out=DK, in_=OH4.rearrange("p t k n -> p (t k) n"), axis=AX.X, op=ALU.add), reads=[R], writes=[R])
            S.op('dve', lambda e: e.tensor_copy(out=IDX[:, 32 * bt:32 * bt + 32], in_=DK), reads=[R], writes=[r_idx])
            S.op('dve', lambda e: e.tensor_tensor(out=D4.rearrange("p (t k) -> p t k", k=4), in0=fap(T8, i0_ * 8, [[8, 8], [1, 4]]),
                                                  in1=fap(T8, i0_ * 8, [[8, 8], [0, 4]]), op=ALU.subtract),
                 reads=lgs, writes=[R])
            S.op('act', lambda e: e.activation(out=D4, in_=D4, func=AF.Exp), reads=[R], writes=[R])
            S.op('dve', lambda e: e.tensor_reduce(out=S4, in_=D4.rearrange("p (t k) -> p t k", k=4), axis=AX.X, op=ALU.add), reads=[R], writes=[R])
            S.op('dve', lambda e: e.reciprocal(out=S4, in_=S4), reads=[R], writes=[R])
            S.op('dve', lambda e: e.tensor_tensor(out=GK[:, 32 * bt:32 * bt + 32].rearrange("p (t k) -> p t k", k=4),
                                                  in0=D4.rearrange("p (t k) -> p t k", k=4), in1=fap(S4, 0, [[1, 8], [0, 4]]), op=ALU.mult),
                 reads=[R], writes=[r_idx])
            for il in range(8):
                i = i0_ + il
                for k in range(4):
                    S.dma('pool', lambda e, k=k, i=i: e.indirect_dma_start(
                        out=xs_d, out_offset=bass.IndirectOffsetOnAxis(ap=IDX[:, 4 * i + k:4 * i + k + 1], axis=0),
                        in_=X1H[:, i, :], in_offset=None, bounds_check=bcreg(e), oob_is_err=False),
                        f'scat{bt}', reads=[r_x1h[i], r_idx], writes=[r_xs], nowaw='any')

        stage1(0)
        stage1(1)
        for i in range(16):
            if i + 2 < 16:
                stage1(i + 2)
            stage2(i)
            if i == 7:
                batch(0)
        batch(1)
        dump("LG", LG, r_lg, [128, 16, NE])
        dump("IDX", IDX, [r_idx], [128, 64], I32)
        dump("GK", GK, [r_idx], [128, 64])
        S.barrier()
        cur[0] = mark2
        if stop == 'router':
            final = []
            for k, v in S.cnt.items():
                if k.startswith('dbg'):
                    final.append(('d', k, v, 'sp'))
            S.wait_all('sp', final)
            S.emit()
            return nc, dbg_out

        NRING = 8
        Wr = [alloc([8, DM], BF16) for _ in range(NRING)]
        r_Wr = [S.res() for _ in range(NRING)]
        bgt = alloc([2 * NE * 8], F32)
        bu1 = alloc([NE * 8], F32)
        r_bg = S.res()
        xsb = [alloc([3, DM], BF16) for _ in range(2)]
        r_xsb = [S.res() for _ in range(2)]
        xsT = [alloc([8, CAP], BF16) for _ in range(2)]
        r_xsT = [S.res() for _ in range(2)]
        hT = alloc([8, CAP], BF16)
        r_hT = S.res()
        gc = [alloc([CAP], F32) for _ in range(2)]
        sgm = [alloc([CAP], F32) for _ in range(2)]
        u1 = [alloc([CAP], F32) for _ in range(2)]
        r_ew = [S.res() for _ in range(2)]
        yo = [alloc([DM], F32) for _ in range(3)]
        r_yo = [S.res() for _ in range(3)]
        r_ys = S.res("ys_scr")
        bdb = [alloc([DM], F32) for _ in range(2)]
        r_bdb = [S.res() for _ in range(2)]

        def load_bd(e_):
            s_ = e_ % 2
            S.dma('sp', lambda e, s_=s_, e_=e_: e.dma_start(out=bdb[s_], in_=bd_d[e_:e_ + 1, :].partition_broadcast(128)), f'ldbd{s_}', writes=[r_bdb[s_]])

        wsrc = [wg_d.rearrange("(e p) f -> p e f", p=128), wu_d.rearrange("(e p) f -> p e f", p=128),
                wd_d.rearrange("(e p) f -> p e f", p=128)]
        xs_v = xs_d.rearrange("(e s p) d -> p e s d", p=128, s=3)
        ycount = [0]

        def load_mat(n):
            if n >= 3 * NE:
                return
            e_, m, sl_ = n // 3, n % 3, n % NRING
            S.dma('pool', lambda e, m=m, sl_=sl_, e_=e_: e.dma_start(out=Wr[sl_].rearrange("p k n -> p (k n)"), in_=wsrc[m][:, e_], max_dma_last_dim=4096), f'ldW{sl_}', writes=[r_Wr[sl_]])

        def load_xs(e_):
            s_ = e_ % 2
            S.dma('sp', lambda e, s_=s_, e_=e_: e.dma_start(out=xsb[s_], in_=xs_v[:, e_]), f'ldxs{s_}', reads=[r_xs], writes=[r_xsb[s_]])

        def transposes(e_):
            s_ = e_ % 2
            for stl in range(3):
                b = stl % 2
                for kc in range(8):
                    S.op('pe', lambda e, b=b, kc=kc, stl=stl, s_=s_: e.transpose(bankb(b)[:, 128 * kc:128 * kc + 128], xsb[s_][:, stl, 128 * kc:128 * kc + 128], identb),
                         reads=[r_xsb[s_], r_const], writes=[rB[b]], acc=(kc > 0))
                S.op('act' if stl != 1 else 'dve',
                     (lambda e, b=b, stl=stl, s_=s_: e.copy(out=xsT[s_][:, :, 128 * stl:128 * stl + 128], in_=bankb(b).rearrange("p (k t) -> p k t", t=128))) if stl != 1 else
                     (lambda e, b=b, stl=stl, s_=s_: e.tensor_copy(out=xsT[s_][:, :, 128 * stl:128 * stl + 128], in_=bankb(b).rearrange("p (k t) -> p k t", t=128))),
                     reads=[rB[b]], writes=[r_xsT[s_]])

        for n in range(NRING):
            load_mat(n)
        S.dma('sp', lambda e: e.dma_start(out=bgt, in_=bgu), 'ldbg', writes=[r_bg])
        S.op('dve', lambda e: e.tensor_scalar(out=bu1, in0=bgt[:, NE * 8:2 * NE * 8], scalar1=1.0, scalar2=None, op0=ALU.add), reads=[r_bg], writes=[r_bg])
        load_xs(0)
        load_xs(1)
        load_bd(0)
        load_bd(1)
        transposes(0)
        for e_ in range(NE):
            s_ = e_ % 2
            Wg_, Wu_, Wd_ = Wr[(3 * e_) % NRING], Wr[(3 * e_ + 1) % NRING], Wr[(3 * e_ + 2) % NRING]
            rWg, rWu, rWd = r_Wr[(3 * e_) % NRING], r_Wr[(3 * e_ + 1) % NRING], r_Wr[(3 * e_ + 2) % NRING]
            if e_ == 0:
                dump("xsT0", xsT[0], [r_xsT[0]], [128, 8, CAP], BF16)
            for fc in range(8):
                a = fc % 2
                bG, bU = 2 + 2 * a, 3 + 2 * a
                for kc in range(8):
                    S.op('pe', lambda e, kc=kc, fc=fc, bG=bG, s_=s_, Wg_=Wg_: e.matmul(bank(bG)[:, 0:CAP], lhsT=Wg_[:, kc, 128 * fc:128 * fc + 128], rhs=xsT[s_][:, kc, :],
                                                                                      start=(kc == 0), stop=(kc == 7)),
                         reads=[rWg, r_xsT[s_]], writes=[rB[bG]], acc=(kc > 0))
                for kc in range(8):
                    S.op('pe', lambda e, kc=kc, fc=fc, bU=bU, s_=s_, Wu_=Wu_: e.matmul(bank(bU)[:, 0:CAP], lhsT=Wu_[:, kc, 128 * fc:128 * fc + 128], rhs=xsT[s_][:, kc, :],
                                                                                      start=(kc == 0), stop=(kc == 7)),
                         reads=[rWu, r_xsT[s_]], writes=[rB[bU]], acc=(kc > 0))
                bi = e_ * 8 + fc
                S.op('dve', lambda e, a=a, bG=bG, bi=bi: e.tensor_scalar(out=gc[a], in0=bank(bG)[:, 0:CAP], scalar1=bgt[:, bi:bi + 1], scalar2=7.0,
                                                                        op0=ALU.add, op1=ALU.min),
                     reads=[rB[bG], r_bg], writes=[r_ew[a]])
                S.op('act', lambda e, a=a: e.activation(out=sgm[a], in_=gc[a], func=AF.Silu, scale=1.702), reads=[r_ew[a]], writes=[r_ew[a]])
                S.op('dve', lambda e, a=a, bU=bU, bi=bi: e.tensor_scalar(out=u1[a], in0=bank(bU)[:, 0:CAP], scalar1=bu1[:, bi:bi + 1], scalar2=8.0,
                                                                        op0=ALU.add, op1=ALU.min),
                     reads=[rB[bU], r_bg], writes=[r_ew[a]])
                S.op('dve', lambda e, a=a, fc=fc: e.scalar_tensor_tensor(out=hT[:, fc, :], in0=u1[a], scalar=-6.0, in1=sgm[a], op0=ALU.max, op1=ALU.mult),
                     reads=[r_ew[a]], writes=[r_hT])
            load_mat(3 * e_ + NRING)
            load_mat(3 * e_ + 1 + NRING)
            if e_ == 0:
                dump("hT0", hT, [r_hT], [128, 8, CAP], BF16)
            if e_ + 1 < NE:
                transposes(e_ + 1)
            if e_ + 2 < NE:
                load_xs(e_ + 2)
            for stl in range(3):
                yi = ycount[0] % 3
                ycount[0] += 1
                for h_ in range(2):
                    b = 6 + h_
                    for fc in range(8):
                        S.op('pe', lambda e, fc=fc, b=b, h_=h_, stl=stl, Wd_=Wd_: e.matmul(bank(b), lhsT=hT[:, fc, 128 * stl:128 * stl + 128],
                                                                                          rhs=Wd_[:, fc, 512 * h_:512 * h_ + 512],
                                                                                          start=(fc == 0), stop=(fc == 7)),
                             reads=[r_hT, rWd], writes=[rB[b]], acc=(fc > 0))
                    S.op('dve', lambda e, b=b, h_=h_, yi=yi, s_=s_: e.scalar_tensor_tensor(out=yo[yi][:, 512 * h_:512 * h_ + 512], in0=bank(b), scalar=1.0 / 1.702,
                                                                                          in1=bdb[s_][:, 512 * h_:512 * h_ + 512], op0=ALU.mult, op1=ALU.add),
                         reads=[rB[b], r_bdb[s_]], writes=[r_yo[yi]])
                if e_ == 0 and stl == 0:
                    dump("yo0", yo[yi], [r_yo[yi]], [128, DM])
                row0 = e_ * CAP + 128 * stl
                S.dma('sp', lambda e, yi=yi, row0=row0: e.dma_start(out=ys_d[row0:row0 + 128, :], in_=yo[yi]), f'sty{yi}',
                      reads=[r_yo[yi]], writes=[r_ys], nowaw='any')
            load_mat(3 * e_ + 2 + NRING)
            if e_ + 2 < NE:
                load_bd(e_ + 2)
        S.barrier()
        cur[0] = mark2

        lnv2 = alloc([4, DM], F32)
        r_lnv2 = S.res()
        x1c = [alloc([DM], F32) for _ in range(3)]
        r_x1c = [S.res() for _ in range(3)]
        yk = [[alloc([DM], F32) for _ in range(4)] for _ in range(3)]
        r_yk = [[S.res() for _ in range(4)] for _ in range(3)]
        acc_ = [alloc([DM], F32) for _ in range(2)]
        r_acc = [S.res() for _ in range(2)]
        ob_ = [alloc([DM], F32) for _ in range(2)]
        r_ob = [S.res() for _ in range(2)]
        lnt2 = [(alloc([12], F32), alloc([2], F32), alloc([1], F32), alloc([1], F32), S.res()) for _ in range(2)]
        S.dma('sp', lambda e: e.dma_start(out=lnv2, in_=lnvec.rearrange("p (a b) -> p a b", b=DM)), 'ldln2', writes=[r_lnv2])
        out_toks = []

        def fetch(i):
            c3 = i % 3
            tsl = slice(128 * i, 128 * i + 128)
            S.dma('sp', lambda e, c3=c3, tsl=tsl: e.dma_start(out=x1c[c3], in_=x1_d[tsl, :]), f'ldx1{c3}', reads=[r_x1d[i]], writes=[r_x1c[c3]])
            for k in range(4):
                S.dma('pool', lambda e, c3=c3, k=k, i=i: e.indirect_dma_start(
                    out=yk[c3][k], out_offset=None, in_=ys_d,
                    in_offset=bass.IndirectOffsetOnAxis(ap=IDX[:, 4 * i + k:4 * i + k + 1], axis=0),
                    bounds_check=bcreg(e), oob_is_err=False),
                    f'gat{c3}{k}', reads=[r_ys, r_idx], writes=[r_yk[c3][k]])

        def accum(i):
            a = i % 2
            c3 = i % 3
            S.op('act', lambda e, a=a, c3=c3: e.activation(out=acc_[a], in_=x1c[c3], func=AF.Copy, scale=ALPHA), reads=[r_x1c[c3]], writes=[r_acc[a]])

        def accum2(i):
            a = i % 2
            c3 = i % 3
            for k in range(4):
                S.op('dve', lambda e, a=a, k=k, i=i, c3=c3: e.scalar_tensor_tensor(out=acc_[a], in0=yk[c3][k], scalar=GK[:, 4 * i + k:4 * i + k + 1], in1=acc_[a],
                                                                                  op0=ALU.mult, op1=ALU.add),
                     reads=[r_yk[c3][k], r_idx, r_acc[a]], writes=[r_acc[a]])

        def ln2_stats(i):
            a = i % 2
            src, rsrc = acc_[a], r_acc[a]
            bst, mv, rs1, nmr, r_ln = lnt2[i % 2]
            S.op('dve', lambda e: e.bn_stats(out=bst[:, 0:6], in_=src[:, 0:512]), reads=[rsrc], writes=[r_ln])
            S.op('dve', lambda e: e.bn_stats(out=bst[:, 6:12], in_=src[:, 512:1024]), reads=[rsrc], writes=[r_ln])
            S.op('dve', lambda e: e.bn_aggr(out=mv, in_=bst), reads=[r_ln], writes=[r_ln])
            S.op('dve', lambda e: e.tensor_scalar(out=rs1, in0=mv[:, 1:2], scalar1=LN_EPS, scalar2=None, op0=ALU.add), reads=[r_ln], writes=[r_ln])
            S.op('act', lambda e: e.activation(out=rs1, in_=rs1, func=AF.Sqrt), reads=[r_ln], writes=[r_ln])
            S.op('dve', lambda e: e.reciprocal(out=rs1, in_=rs1), reads=[r_ln], writes=[r_ln])
            S.op('dve', lambda e: e.tensor_scalar(out=nmr, in0=mv[:, 0:1], scalar1=rs1, scalar2=-1.0, op0=ALU.mult, op1=ALU.mult),
                 reads=[r_ln], writes=[r_ln])
            S.op('act', lambda e: e.activation(out=src, in_=src, func=AF.Identity, bias=nmr, scale=rs1), reads=[rsrc, r_ln], writes=[rsrc])

        def ln2_gb(i):
            a = i % 2
            tsl = slice(128 * i, 128 * i + 128)
            src, rsrc = acc_[a], r_acc[a]
            S.op('dve', lambda e: e.tensor_tensor(out=src, in0=src, in1=lnv2[:, 2, :], op=ALU.mult), reads=[rsrc, r_lnv2], writes=[rsrc])
            S.op('dve', lambda e: e.tensor_tensor(out=ob_[a], in0=src, in1=lnv2[:, 3, :], op=ALU.add), reads=[rsrc, r_lnv2], writes=[r_ob[a]])
            out_toks.append(S.dma('sp', lambda e, a=a, tsl=tsl: e.dma_start(out=out_d[tsl, :], in_=ob_[a]), f'sto{a}', reads=[r_ob[a]]))

        fetch(0)
        fetch(1)
        fetch(2)
        accum(0)
        accum2(0)
        for i in range(16):
            if i + 3 < 16:
                fetch(i + 3)
            if i + 1 < 16:
                accum(i + 1)
            ln2_stats(i)
            if i + 1 < 16:
                accum2(i + 1)
            ln2_gb(i)
        final = [t for t in out_toks]
        for k, v in S.cnt.items():
            if k.startswith('dbg'):
                final.append(('d', k, v, 'sp'))
        S.wait_all('sp', final)
        S.emit()
    return nc, dbg_out


def _inv_freq():
    e = 64
    try:
        import jax
        import jax.numpy as jnp
        with jax.default_device(jax.devices('cpu')[0]):
            v = 10000.0 ** (-jnp.arange(0, e, 2, dtype=jnp.float32) / e)
            return np.asarray(v, dtype=np.float32)
    except Exception:
        return (np.float32(10000.0) ** (-np.arange(0, e, 2, dtype=np.float32) / np.float32(e))).astype(np.float32)


def make_in_maps(x, positions, w_in, conv_w, conv_b, conv_ln_g, conv_ln_b, conv_pw_w, conv_pw_b, w_out, ln1_g, ln1_b,
                 router_w, router_b, exp_w_gate, exp_b_gate, exp_w_up, exp_b_up, exp_w_down, exp_b_down, ln2_g, ln2_b):
    f = lambda a: np.ascontiguousarray(np.asarray(a, dtype=np.float32))
    x = f(x)
    positions = np.asarray(positions).astype(np.int32)

    def p4(v):
        return f(v).reshape(4, 128).T
    cvec = np.ascontiguousarray(np.concatenate([p4(conv_b[0]), p4(conv_ln_g[0]), p4(conv_ln_b[0]), p4(conv_pw_b[0])], axis=1))
    convw = np.ascontiguousarray(f(conv_w[0]).T.reshape(4, 128, 31).transpose(1, 0, 2).reshape(128, 124))
    lnvec = np.ascontiguousarray(np.broadcast_to(np.concatenate([f(ln1_g[0]), f(ln1_b[0]), f(ln2_g[0]), f(ln2_b[0])])[None, :], (128, 4 * DM)))
    rbr = np.ascontiguousarray(np.broadcast_to(f(router_b[0])[None, :], (128, NE)))
    bg = f(exp_b_gate[0]).reshape(NE, 8, 128).transpose(2, 0, 1).reshape(128, NE * 8)
    bu = f(exp_b_up[0]).reshape(NE, 8, 128).transpose(2, 0, 1).reshape(128, NE * 8)
    bgu = np.ascontiguousarray(np.concatenate([bg, bu], axis=1))
    invf = _inv_freq()
    pidx = np.arange(128)

    def wtile(w):
        return np.ascontiguousarray(f(w).reshape(NE, 8, 128, DM).transpose(0, 2, 1, 3)).reshape(NE * 128, 8 * DM)
    shared = dict(
        w_in=f(w_in[0]), convw=convw, cvec=cvec, pw_w=f(conv_pw_w[0]), w_out=f(w_out[0]), lnvec=lnvec,
        rw=f(router_w[0]), rbr=rbr, wg=wtile(exp_w_gate[0]), wu=wtile(exp_w_up[0]),
        wd=wtile(exp_w_down[0]), bgu=bgu, bd=f(exp_b_down[0]),
        zsrc=np.zeros((256, DM), dtype=ml_dtypes.bfloat16),
    )
    maps = []
    for c in range(NCORES):
        b, qi = c // 4, c % 4
        t0 = TOK * qi
        xTc = np.zeros((DM, TT), np.float32)
        posc = np.zeros((1, TT), np.int32)
        if qi > 0:
            xTc[:, :TOK] = x[b, t0 - TOK:t0].T
            posc[0, :TOK] = positions[b, t0 - TOK:t0]
        xTc[:, TOK:] = x[b, t0:t0 + TOK].T
        posc[0, TOK:] = positions[b, t0:t0 + TOK]
        cstc = np.zeros((128, 4), np.float32)
        cstc[:, 0] = invf[pidx % 32]
        cstc[:, 1] = np.where((pidx % 64) < 32, -1.0, 1.0)
        cstc[:, 2] = 1.0 if qi > 0 else 0.0
        m = dict(shared)
        xTc = np.ascontiguousarray(xTc.reshape(8, 128, 8, 512).transpose(2, 1, 0, 3)).reshape(8, 128, 8 * 512)
        m.update(xT=xTc, xtok=np.ascontiguousarray(x[b, t0:t0 + TOK]), pos=posc, cst=cstc)
        maps.append(m)
    return maps


_NC_CACHE = {}


def kernel(**inputs):
    if 'nc' not in _NC_CACHE:
        _NC_CACHE['nc'] = build_program()[0]
    nc = _NC_CACHE['nc']
    in_maps = make_in_maps(**inputs)
    res = run_bass_kernel_spmd(nc, in_maps, core_ids=list(range(NCORES)))
    out = np.zeros((2, 8192, DM), np.float32)
    for c in range(NCORES):
        b, qi = c // 4, c % 4
        out[b, TOK * qi:TOK * qi + TOK] = np.asarray(res.results[c]["out"], dtype=np.float32)
    return out
```

```python
import math
import numpy as np
import ml_dtypes
from contextlib import ExitStack
import concourse.bass as bass
import concourse.mybir as mybir
from concourse.bass_utils import run_bass_kernel_spmd

F32 = mybir.dt.float32
BF16 = mybir.dt.bfloat16
I32 = mybir.dt.int32
ALU = mybir.AluOpType
AF = mybir.ActivationFunctionType
AX = mybir.AxisListType

NCORES = 8
TOK = 2048
TT = 4096
DM = 1024
NE = 32
CAP = 384
NSLOT = NE * CAP
ALPHA = float(2.0 ** 0.25)
LN_EPS = 1e-5
PI = math.pi
ENG = ['pe', 'act', 'dve', 'pool', 'sp']


class Res:
    __slots__ = ('name', 'w', 'r')

    def __init__(self, name):
        self.name = name
        self.w = None
        self.r = []


class Sched:
    def __init__(self, nc, stack):
        self.nc = nc
        self.stack = stack
        self.ops = {e: [] for e in ENG}
        self.cnt = {}
        self.seen = {e: {} for e in ENG}
        self.sems = {}
        self.clk = {}
        self.nres = 0

    def res(self, name=None):
        self.nres += 1
        return Res(name or f"r{self.nres}")

    def _tok_kv(self, tok):
        if tok[0] == 'd':
            return tok[1], tok[2]
        _, eng, idx = tok
        ops = self.ops[eng]
        for j in range(idx, len(ops)):
            s = ops[j][2]
            if s is not None and s[0] == eng:
                return eng, s[2]
        j = len(ops) - 1
        while ops[j][3] != 'c':
            j -= 1
        assert j >= idx
        v = self.cnt.get(eng, 0) + 1
        self.cnt[eng] = v
        ops[j][2] = [eng, 1, v]
        self.clk[(eng, v)] = ops[j][4]
        return eng, v

    def _need(self, eng, waits, tok, raw):
        if tok is None:
            return
        if tok[0] == 'c' and tok[1] == eng and not raw:
            return
        k, v = self._tok_kv(tok)
        if self.seen[eng].get(k, 0) >= v:
            return
        if waits.get(k, 0) < v:
            waits[k] = v

    def _commit_waits(self, eng, waits):
        seen = self.seen[eng]
        for k, v in waits.items():
            if seen.get(k, 0) < v:
                seen[k] = v
            c = self.clk.get((k, v))
            if c:
                for k2, v2 in c.items():
                    if seen.get(k2, 0) < v2:
                        seen[k2] = v2

    def op(self, eng, fn, reads=(), writes=(), acc=False):
        waits = {}
        for r in reads:
            self._need(eng, waits, r.w, True)
        if not acc:
            for w in writes:
                self._need(eng, waits, w.w, False)
                for t in w.r:
                    self._need(eng, waits, t, False)
        self._commit_waits(eng, waits)
        snap = dict(self.seen[eng])
        idx = len(self.ops[eng])
        self.ops[eng].append([waits, fn, None, 'c', snap])
        tok = ('c', eng, idx)
        for r in reads:
            r.r.append(tok)
        for w in writes:
            w.w = tok
            if not acc:
                w.r = []
        return tok

    def dma(self, eng, fn, sem, reads=(), writes=(), nowaw=False):
        waits = {}
        for r in reads:
            self._need(eng, waits, r.w, True)
        for w in writes:
            if not (nowaw == 'any' or (nowaw and w.w is not None and w.w[0] == 'd' and w.w[1] == sem)):
                self._need(eng, waits, w.w, True)
            for t in w.r:
                self._need(eng, waits, t, True)
        self._commit_waits(eng, waits)
        v = self.cnt.get(sem, 0) + 16
        self.cnt[sem] = v
        snap = dict(self.seen[eng])
        self.clk[(sem, v)] = snap
        self.ops[eng].append([waits, fn, [sem, 16, v], 'd', snap])
        tok = ('d', sem, v, eng)
        for r in reads:
            r.r.append(tok)
        for w in writes:
            w.w = tok
            w.r = []
        return tok

    def wait_all(self, eng, toks):
        waits = {}
        for t in toks:
            self._need(eng, waits, t, True)
        self._commit_waits(eng, waits)
        self.ops[eng].append([waits, None, None, 'w', None])

    def barrier(self, exclude=()):
        toks = []
        for e in ENG:
            ops = self.ops[e]
            for j in range(len(ops) - 1, -1, -1):
                if ops[j][3] == 'c':
                    toks.append(('c', e, j))
                    break
        for k, v in list(self.cnt.items()):
            if k not in ENG and not any(k.startswith(x) for x in exclude):
                toks.append(('d', k, v, None))
        for e in ENG:
            self.wait_all(e, toks)

    def emit(self):
        nc = self.nc
        for k in sorted(self.cnt.keys()):
            self.sems[k] = self.stack.enter_context(nc.semaphore("s_" + k))
        engs = {'pe': 'tensor', 'act': 'scalar', 'dve': 'vector', 'pool': 'gpsimd', 'sp': 'sync'}
        sems = self.sems
        with nc.Block() as block:
            for e in ENG:
                ops = self.ops[e]

                def body(engine, ops=ops):
                    for waits, fn, sig, kind, _ in ops:
                        for k, v in waits.items():
                            engine.wait_ge(sems[k], v)
                        if fn is None:
                            continue
                        ins = fn(engine)
                        if sig is not None:
                            ins.then_inc(sems[sig[0]], sig[1])
                getattr(block, engs[e])(body)


_REG = {}


def bcreg(e):
    if 'bc' not in _REG:
        _REG['bc'] = e.to_reg(NSLOT - 1)
    return _REG['bc']


def strided(ap, start, step, n):
    pat = [list(x) for x in ap.ap]
    es = pat[-1][0]
    return bass.AP(ap.tensor, ap.offset + start * es, pat[:-1] + [[step * es, n]])


def build_program(debug=None, stop=None):
    debug = debug or []
    nc = bass.Bass("TRN2", target_bir_lowering=False)
    _REG.clear()
    st = ExitStack()
    S = Sched(nc, st)

    def din(name, shape, dt=F32):
        return nc.dram_tensor(name, list(shape), dt, kind="ExternalInput").ap()

    xT = din("xT", [8, 128, 8 * 512])
    xtok = din("xtok", [TOK, DM])
    posd = din("pos", [1, TT], I32)
    w_in = din("w_in", [DM, 2560])
    convw = din("convw", [128, 4 * 31])
    cvec = din("cvec", [128, 16])
    pw_w = din("pw_w", [512, 512])
    w_out = din("w_out", [DM, DM])
    lnvec = din("lnvec", [128, 4 * DM])
    rw = din("rw", [DM, NE])
    rbr = din("rbr", [128, NE])
    if stop:
        wg_d = wu_d = wd_d = None
    else:
        wg_d = din("wg", [NE * 128, 8 * DM])
        wu_d = din("wu", [NE * 128, 8 * DM])
        wd_d = din("wd", [NE * 128, 8 * DM])
    bgu = din("bgu", [128, 2 * NE * 8])
    bd_d = din("bd", [NE, DM])
    cst = din("cst", [128, 4])
    zsrc = din("zsrc", [256, DM], BF16)
    out_d = nc.dram_tensor("out", [TOK, DM], F32, kind="ExternalOutput").ap()
    xs_d = nc.dram_tensor("xs_scr", [NSLOT, DM], BF16, kind="Internal").ap()
    ys_d = nc.dram_tensor("ys_scr", [NSLOT, DM], F32, kind="Internal").ap()
    x1_d = nc.dram_tensor("x1_scr", [TOK, DM], F32, kind="Internal").ap()
    dbg_out = {}

    ARENA = 207 * 1024
    with st:
        A = st.enter_context(nc.sbuf_tensor("arena", [128, ARENA // 2], BF16))
        PS = st.enter_context(nc.psum_tensor("PS", [128, 4096], F32))
        PSb = PS[:].bitcast(BF16)

        cur = [0]

        def alloc(shape, dt, name=None):
            n = int(np.prod(shape))
            nb = n * mybir.dt.size(dt)
            nb = (nb + 63) // 64 * 64
            off = cur[0]
            cur[0] += nb
            assert cur[0] <= ARENA, (name, cur[0])
            ap = A[:, off // 2: off // 2 + n * mybir.dt.size(dt) // 2]
            if dt != BF16:
                ap = ap.bitcast(dt)
            if len(shape) == 2:
                ap = ap.rearrange("p (a b) -> p a b", b=shape[1])
            elif len(shape) == 3:
                ap = ap.rearrange("p (a b c) -> p a b c", b=shape[1], c=shape[2])
            return ap

        def bank(b):
            return PS[:, 512 * b:512 * b + 512]

        def bankb(b):
            return PSb[:, 1024 * b:1024 * b + 1024]
        rB = [S.res(f"bank{b}") for b in range(8)]

        ndbg = [0]

        def dump(name, ap, res, shape, dt=F32):
            if name not in debug:
                return
            t = nc.dram_tensor("dbg_" + name, list(shape), dt, kind="ExternalOutput").ap()
            dbg_out[name] = t
            ndbg[0] += 1
            S.dma('sp', lambda e: e.dma_start(out=t, in_=ap), f'dbg{ndbg[0]}', reads=res)

        identf = alloc([128], F32)
        identb = alloc([128], BF16)
        onesb = alloc([128], BF16)
        onesf = alloc([128], F32)
        trib = alloc([128], BF16)
        M1 = alloc([512], BF16)
        M1f = alloc([512], BF16)
        M2f = alloc([512], BF16)
        M3 = alloc([4, 512], BF16)
        cstt = alloc([4], F32)
        ecf = alloc([NE], F32)
        r_const = S.res("const")
        r_idx = S.res("idx")
        mcf = alloc([128], F32)
        mpf_ = alloc([128], F32)
        mpff = alloc([128], F32)
        eci = alloc([NE], I32)
        xt_mark = cur[0]
        XT = alloc([8, 8, 512], BF16, "XT")
        rXT = [S.res(f"xt{i}") for i in range(8)]
        attnT_mark = cur[0]
        attnT = alloc([4, TOK], BF16, "attnT")
        r_attnT = [S.res(f"attnT{g}") for g in range(4)]
        persist_mark = cur[0]

        def load_xt(i):
            S.dma('pool', lambda e, i=i: e.dma_start(out=XT[:, i].rearrange("p k t -> p (k t)"), in_=xT[i], max_dma_last_dim=8192),
                  f'ldxt{i}', writes=[rXT[i]])
        r_cst = S.res("cst")
        S.dma('sp', lambda e: e.dma_start(out=cstt, in_=cst), 'ldc', writes=[r_cst])

        def build_consts():
            def aff(out, pattern, cm, op):
                S.op('pool', lambda e: e.memset(out, 1.0), writes=[r_const])
                S.op('pool', lambda e: e.affine_select(out=out, in_=out, pattern=pattern, compare_op=op, fill=0.0,
                                                       base=0, channel_multiplier=cm), reads=[r_const], writes=[r_const])
            aff(identf, [[-1, 128]], 1, ALU.is_equal)
            aff(mcf, [[1, 128]], -1, ALU.is_ge)
            aff(mpf_, [[-1, 128]], 1, ALU.is_ge)
            S.op('pool', lambda e: e.tensor_copy(out=identb, in_=identf), reads=[r_const], writes=[r_const])
            S.op('pool', lambda e: e.memset(onesb, 1.0), writes=[r_const])
            S.op('pool', lambda e: e.memset(onesf, 1.0), writes=[r_const])
            S.op('pool', lambda e: e.tensor_tensor(out=trib, in0=mcf, in1=identf, op=ALU.subtract), reads=[r_const], writes=[r_const])
            S.op('dve', lambda e: e.tensor_scalar(out=mpff, in0=mpf_, scalar1=cstt[:, 2:3], scalar2=None, op0=ALU.mult),
                 reads=[r_const, r_cst], writes=[r_const])
            for (M, parts) in ((M1, [mpf_, mcf, mpf_, mcf]), (M1f, [mpff, mcf, mpf_, mcf]), (M2f, [mpff, mcf, mpff, mcf])):
                for s_, src in enumerate(parts):
                    S.op('pool', lambda e, M=M, s_=s_, src=src: e.tensor_copy(out=M[:, 128 * s_:128 * s_ + 128], in_=src),
                         reads=[r_const], writes=[r_const])
            for tb in range(4):
                for r_ in range(8):
                    S.op('pool', lambda e, tb=tb, r_=r_: e.tensor_copy(out=M3[:, tb, 64 * r_:64 * r_ + 32], in_=mpff[:, 32 * tb:32 * tb + 32]),
                         reads=[r_const], writes=[r_const])
                    S.op('pool', lambda e, tb=tb, r_=r_: e.tensor_copy(out=M3[:, tb, 64 * r_ + 32:64 * r_ + 64], in_=mcf[:, 32 * tb:32 * tb + 32]),
                         reads=[r_const], writes=[r_const])
            S.op('pool', lambda e: e.iota(eci, pattern=[[CAP, NE]], base=0, channel_multiplier=0), writes=[r_const])
            S.op('pool', lambda e: e.tensor_copy(out=ecf, in_=eci), reads=[r_const], writes=[r_const])
        r_xs = S.res("xs_scr")

        cosT = alloc([TT], F32, "cos")
        sinT = alloc([TT], F32, "sin")
        r_tab = S.res("tab")
        wgt = alloc([8, 5, 128], BF16, "wgt")
        r_wgt = S.res("wgt")
        QT = alloc([TOK], BF16)
        KT = alloc([TT], BF16)
        VT = alloc([TT], BF16)
        r_QT = [S.res() for _ in range(4)]
        r_KT = [S.res() for _ in range(8)]
        r_VT = [S.res() for _ in range(8)]
        NKB = 69
        VTOK = alloc([NKB, 192], BF16)
        r_vtok = S.res("vtok")
        tmp_mark = cur[0]
        Ebuf = [alloc([512], BF16) for _ in range(3)]
        r_E = [S.res() for _ in range(3)]
        PTb = [alloc([512], BF16) for _ in range(3)]
        r_PT = [S.res() for _ in range(3)]
        lnb = [alloc([512], F32) for _ in range(2)]
        r_lnb = [S.res() for _ in range(2)]
        rdb = [alloc([512], F32) for _ in range(2)]
        r_rdb = [S.res() for _ in range(2)]
        alias_end = cur[0]
        rt = [alloc([512], F32) for _ in range(4)]
        r_rt = [S.res() for _ in range(4)]
        att_mark = cur[0]

        cur[0] = tmp_mark
        posi = alloc([512], I32)
        ang = alloc([512], F32)
        a2 = alloc([512], F32)
        kfi = alloc([512], I32)
        kff = alloc([512], F32)
        rr2 = [alloc([512], F32) for _ in range(2)]
        assert cur[0] <= alias_end, (cur[0], alias_end)
        r_pos = S.res("tabpos")
        r_tmp = S.res("tabtmp")
        r_rr = [S.res() for _ in range(2)]

        def load_pos(i):
            sl = slice(512 * i, 512 * i + 512)
            S.dma('sp', lambda e, sl=sl: e.dma_start(out=posi, in_=posd[:, sl].partition_broadcast(128)), 'ldpos', writes=[r_pos])

        def gen_tab(i):
            sl = slice(512 * i, 512 * i + 512)
            S.op('dve', lambda e: e.tensor_copy(out=ang, in_=posi), reads=[r_pos], writes=[r_tmp])
            if i + 1 < 8:
                load_pos(i + 1)
            S.op('dve', lambda e: e.tensor_scalar(out=ang, in0=ang, scalar1=cstt[:, 0:1], scalar2=None, op0=ALU.mult),
                 reads=[r_tmp, r_cst], writes=[r_tmp])
            for ti, (tab, shift, scl) in enumerate(((sinT, 0.0, cstt[:, 1:2]), (cosT, PI / 2, 1.0))):
                rr = rr2[ti]
                S.op('dve', lambda e, shift=shift: e.tensor_scalar(out=a2, in0=ang, scalar1=shift, scalar2=None, op0=ALU.add),
                     reads=[r_tmp], writes=[r_tmp])
                S.op('dve', lambda e: e.tensor_scalar(out=kfi, in0=a2, scalar1=1.0 / (2 * PI), scalar2=None, op0=ALU.mult),
                     reads=[r_tmp], writes=[r_tmp])
                S.op('dve', lambda e: e.tensor_copy(out=kff, in_=kfi), reads=[r_tmp], writes=[r_tmp])
                S.op('dve', lambda e, rr=rr: e.scalar_tensor_tensor(out=rr, in0=kff, scalar=-2 * PI, in1=a2, op0=ALU.mult, op1=ALU.add),
                     reads=[r_tmp], writes=[r_rr[ti]])
                S.op('dve', lambda e, rr=rr: e.tensor_scalar(out=rr, in0=rr, scalar1=-3.14159, scalar2=3.14159, op0=ALU.max, op1=ALU.min),
                     reads=[r_rr[ti]], writes=[r_rr[ti]])
                S.op('act', lambda e, tab=tab, sl=sl, scl=scl, rr=rr: e.activation(out=tab[:, sl], in_=rr, func=AF.Sin, scale=scl),
                     reads=[r_rr[ti], r_cst], writes=[r_tab])
        def load_wgt(g):
            q0, k0, v0 = 1024 + 128 * g, 1536 + 128 * g, 2048 + 128 * g
            for (slot, c0) in ((0, q0), (2, k0), (4, v0)):
                S.dma('pool', lambda e, slot=slot, c0=c0: e.dma_start(out=wgt[:, :, slot, :], in_=w_in_v[:, :, c0:c0 + 128]),
                      'ldwg', writes=[r_wgt], nowaw=True)
            for (slot, c0) in ((1, q0), (3, k0)):
                for hh in range(2):
                    for half in range(2):
                        d0 = 64 * hh + 32 * half
                        s0 = c0 + 64 * hh + 32 * (1 - half)
                        S.dma('pool', lambda e, slot=slot, d0=d0, s0=s0: e.dma_start(out=wgt[:, :, slot, d0:d0 + 32], in_=w_in_v[:, :, s0:s0 + 32]),
                              'ldwg', writes=[r_wgt], nowaw=True)

        w_in_v = w_in.rearrange("(k p) n -> p k n", p=128)
        load_xt(0)
        load_wgt(0)
        for i in range(1, 8):
            load_xt(i)
        load_pos(0)
        gen_tab(0)
        build_consts()
        S.op('pool', lambda e: e.memset(VTOK[:, :, 64:128], 1.0), writes=[r_vtok])
        cur[0] = att_mark

        w_in_v = w_in.rearrange("(k p) n -> p k n", p=128)

        def kcols(branch, r_, j):
            if branch == 1:
                return (2048 + 128 * j, 1)
            if branch == 2:
                return (2048 + 512 * j + r_, 4)
            return (2048 + 2048 * j + r_, 16)

        def vidx(branch, r_, j):
            if branch == 1:
                return j + 1
            if branch == 2:
                return 17 + r_ * 5 + (j + 1)
            return 37 + r_ * 2 + (j + 1)

        ucount = [0]
        for g in range(4):
            if g > 0:
                load_wgt(g)

            def proj(slot, b, i):
                for kc in range(8):
                    S.op('pe', lambda e, kc=kc: e.matmul(bank(b), lhsT=wgt[:, kc, slot, :], rhs=XT[:, i, kc, :],
                                                       start=(kc == 0), stop=(kc == 7)),
                         reads=[r_wgt, rXT[i]], writes=[rB[b]], acc=(kc > 0))

            pc = [0]

            def rope(dst, rdst, bA, bB, i):
                sl = slice(512 * i, 512 * i + 512)
                a = pc[0] % 2
                pc[0] += 1
                t1, t2 = rt[2 * a], rt[2 * a + 1]
                S.op('dve', lambda e: e.tensor_tensor(out=t1, in0=bank(bA), in1=cosT[:, sl], op=ALU.mult),
                     reads=[rB[bA], r_tab], writes=[r_rt[2 * a]])
                S.op('dve', lambda e: e.tensor_tensor(out=t2, in0=bank(bB), in1=sinT[:, sl], op=ALU.mult),
                     reads=[rB[bB], r_tab], writes=[r_rt[2 * a + 1]])
                S.op('pool', lambda e: e.tensor_tensor(out=dst, in0=t1, in1=t2, op=ALU.add),
                     reads=[r_rt[2 * a], r_rt[2 * a + 1]], writes=[rdst])

            for i in range(8):
                pb = 3 * (i % 2)
                if g == 0 and i + 1 < 8:
                    gen_tab(i + 1)
                proj(2, pb, i)
                proj(3, pb + 1, i)
                rope(KT[:, 512 * i:512 * i + 512], r_KT[i], pb, pb + 1, i)
                proj(4, pb + 2, i)
                S.op('act', lambda e, i=i, pb=pb: e.copy(out=VT[:, 512 * i:512 * i + 512], in_=bank(pb + 2)),
                     reads=[rB[pb + 2]], writes=[r_VT[i]])
                if i >= 4:
                    proj(0, 6, i)
                    proj(1, 7, i)
                    rope(QT[:, 512 * (i - 4):512 * (i - 4) + 512], r_QT[i - 4], 6, 7, i)
            if g == 0:
                dump("KT0", KT, r_KT, [128, TT], BF16)
                dump("QT0", QT, r_QT, [128, TOK], BF16)
                dump("VT0", VT, r_VT, [128, TT], BF16)

            kbs = []
            for j in range(-1, 16):
                kbs.append((1, 0, j))
            for r_ in range(4):
                for j in range(-1, 4):
                    kbs.append((2, r_, j))
            for r_ in range(16):
                for j in range(-1, 1):
                    kbs.append((3, r_, j))
            assert len(kbs) == NKB
            for c0 in range(0, NKB, 8):
                chunk = kbs[c0:c0 + 8]
                b = 6 + (c0 // 8) % 2
                for s_, (br, r_, j) in enumerate(chunk):
                    stt, stp = kcols(br, r_, j)
                    assert vidx(br, r_, j) == c0 + s_
                    S.op('pe', lambda e, b=b, s_=s_, stt=stt, stp=stp: e.transpose(bankb(b)[:, 128 * s_:128 * s_ + 128], strided(VT, stt, stp, 128), identb),
                         reads=r_VT + [r_const], writes=[rB[b]], acc=(s_ > 0))
                n = len(chunk)
                src = bankb(b)[:, 0:128 * n].rearrange("p (n f) -> p n f", f=128)
                S.op('act', lambda e, c0=c0, n=n, src=src: e.copy(out=VTOK[:, c0:c0 + n, 0:64], in_=src[:, :, 0:64]),
                     reads=[rB[b]], writes=[r_vtok])
                S.op('dve', lambda e, c0=c0, n=n, src=src: e.tensor_copy(out=VTOK[:, c0:c0 + n, 128:192], in_=src[:, :, 64:128]),
                     reads=[rB[b]], writes=[r_vtok])

            units = []
            for tb in range(4):
                for hh in range(2):
                    hs = slice(64 * hh, 64 * hh + 64)
                    KTh, QTh = KT[hs, :], QT[hs, :]
                    ob = 4 + (tb * 2 + hh) % 2
                    O = bank(ob)
                    ulist = []
                    for half in range(2):
                        smm, pv = [], []
                        for s_ in range(2):
                            qb = 4 * tb + 2 * half + s_
                            for w_, j in enumerate((qb - 1, qb)):
                                ks, kp = kcols(1, 0, j)
                                col = 256 * s_ + 128 * w_
                                smm.append((strided(KTh, ks, kp, 128), QTh[:, 128 * qb:128 * qb + 128], col, 128))
                                pv.append((vidx(1, 0, j), col, 128, O[:, 128 * (qb - 4 * tb):128 * (qb - 4 * tb) + 128]))
                        mask = M1f if (tb == 0 and half == 0) else M1
                        ulist.append((smm, mask, pv))
                    for half in range(2):
                        smm, pv = [], []
                        for s_ in range(2):
                            r_ = 2 * half + s_
                            for w_, j in enumerate((tb - 1, tb)):
                                ks, kp = kcols(2, r_, j)
                                col = 256 * s_ + 128 * w_
                                smm.append((strided(KTh, ks, kp, 128), strided(QTh, 512 * tb + r_, 4, 128), col, 128))
                                pv.append((vidx(2, r_, j), col, 128, strided(O, r_, 4, 128)))
                        mask = M2f if tb == 0 else M1
                        ulist.append((smm, mask, pv))
                    for half in range(2):
                        smm, pv = [], []
                        for s_ in range(8):
                            r_ = 8 * half + s_
                            for w_, j in enumerate((-1, 0)):
                                ks, kp = kcols(3, r_, j)
                                col = 64 * s_ + 32 * w_
                                smm.append((strided(KTh, ks, kp, 128), strided(QTh, 512 * tb + r_, 16, 32), col, 32))
                                pv.append((vidx(3, r_, j), col, 32, strided(O, r_, 16, 32)))
                        ulist.append((smm, M3[:, tb, :], pv))
                    for ui, u in enumerate(ulist):
                        units.append((tb, hh, ob, ui, u))

            def emit_S(un, sb, eb):
                tb, hh, ob, ui, (smm, mask, pv) = un
                for n_, (lh, rh, col, N) in enumerate(smm):
                    S.op('pe', lambda e, lh=lh, rh=rh, col=col, N=N: e.matmul(bank(sb)[:, col:col + N], lhsT=lh, rhs=rh, start=True, stop=True,
                                                                              skip_group_check=True),
                         reads=r_KT + r_QT, writes=[rB[sb]], acc=(n_ > 0))
                S.op('act', lambda e: e.activation(out=Ebuf[eb], in_=bank(sb), func=AF.Exp, scale=0.125),
                     reads=[rB[sb]], writes=[r_E[eb]])
                eng = 'dve' if (ucount[0] % 3 != 2) else 'pool'
                S.op(eng, lambda e: e.tensor_tensor(out=PTb[eb], in0=Ebuf[eb], in1=mask, op=ALU.mult),
                     reads=[r_E[eb], r_const], writes=[r_PT[eb]])

            def emit_PV(un, eb):
                tb, hh, ob, ui, (smm, mask, pv) = un
                for n_, (vi, col, N, oap) in enumerate(pv):
                    first = (ui == 0 and n_ == 0)
                    last = (ui == 5 and n_ == len(pv) - 1)
                    lh = VTOK[:, vi, 0:128] if hh == 0 else VTOK[:, vi, 64:192]
                    S.op('pe', lambda e, lh=lh, col=col, N=N, oap=oap, first=first, last=last: e.matmul(
                        oap, lhsT=lh, rhs=PTb[eb][:, col:col + N], start=first, stop=last, skip_group_check=True),
                        reads=[r_vtok, r_PT[eb]], writes=[rB[ob]], acc=(not first))
                if ui == 5:
                    num = slice(0, 64) if hh == 0 else slice(64, 128)
                    den = slice(64, 128) if hh == 0 else slice(0, 64)
                    a = (tb * 2 + hh) % 2
                    dsta = attnT[num, g, 512 * tb:512 * tb + 512]
                    S.op('act', lambda e: e.activation(out=lnb[a][den, :], in_=bank(ob)[den, :], func=AF.Ln),
                         reads=[rB[ob]], writes=[r_lnb[a]])
                    S.op('act', lambda e: e.activation(out=rdb[a][den, :], in_=lnb[a][den, :], func=AF.Exp, scale=-1.0),
                         reads=[r_lnb[a]], writes=[r_rdb[a]])
                    S.op('dve', lambda e: e.tensor_tensor(out=dsta, in0=bank(ob)[num, :], in1=rdb[a][den, :], op=ALU.mult),
                         reads=[rB[ob], r_rdb[a]], writes=[r_attnT[g]])

            nun = len(units)
            for u in range(nun + 1):
                if u < nun:
                    emit_S(units[u], u % 4, u % 3)
                    ucount[0] += 1
                if u >= 1:
                    emit_PV(units[u - 1], (u - 1) % 3)
        dump("attnT", attnT, r_attnT, [128, 4, TOK], BF16)
        S.barrier()
        cur[0] = persist_mark

        convoutT = alloc([4, TOK], BF16, "convoutT")
        r_convout = S.res("convout")
        conv_mark = cur[0]
        uT = alloc([4, TOK + 32], BF16)
        r_uT = [S.res() for _ in range(5)]
        diag = alloc([124, 128], BF16)
        r_diag = S.res()
        cwt = alloc([124], F32)
        cvt = alloc([16], F32)
        r_cv = S.res()
        pwb = alloc([4, 512], BF16)
        r_pwb = S.res()
        r_cT = S.res()
        r_csq = S.res()
        sg = [alloc([512], F32) for _ in range(2)]
        r_sg = [S.res() for _ in range(2)]
        mean = alloc([512], F32)
        msq = alloc([512], F32)
        var = alloc([512], F32)
        rstd = alloc([512], F32)
        r_st = S.res()
        tn = [alloc([512], F32) for _ in range(2)]
        r_tn = [S.res() for _ in range(2)]
        sT = alloc([4, 512], BF16)
        r_sT = S.res()
        wag_mark = cur[0]
        wag = alloc([8, 1024], BF16)
        r_wag = S.res()
        cur[0] = wag_mark
        cT = alloc([4, 512], F32)
        csq = alloc([4, 512], F32)

        for h_ in range(2):
            S.dma('pool', lambda e, h_=h_: e.dma_start(out=wag[:, :, 512 * h_:512 * h_ + 512], in_=w_in_v[:, :, 512 * h_:512 * h_ + 512]),
                  'ldwag', writes=[r_wag], nowaw=True)
        S.dma('sp', lambda e: e.dma_start(out=cwt, in_=convw), 'ldcw', writes=[r_cv])
        S.dma('sp', lambda e: e.dma_start(out=cvt, in_=cvec), 'ldcv', writes=[r_cv], nowaw=True)
        S.dma('pool', lambda e: e.dma_start(out=pwb, in_=pw_w.rearrange("(k p) n -> p k n", p=128)), 'ldpw', writes=[r_pwb])
        for c in range(4):
            for j in range(31):
                n_ = c * 31 + j
                S.op('dve', lambda e, n_=n_: e.tensor_scalar(out=diag[:, n_, :], in0=identf, scalar1=cwt[:, n_:n_ + 1], scalar2=None, op0=ALU.mult),
                     reads=[r_cv, r_const], writes=[r_diag])

        def glu(xcols, ucols, ncol, ru, xi):
            for c in range(4):
                bA, bG = 2 * (c % 2), 2 * (c % 2) + 1
                for kc in range(8):
                    S.op('pe', lambda e, kc=kc, c=c, bA=bA: e.matmul(bank(bA)[:, 0:ncol], lhsT=wag[:, kc, 128 * c:128 * c + 128], rhs=XT[:, xi, kc, xcols],
                                                                    start=(kc == 0), stop=(kc == 7)),
                         reads=[r_wag, rXT[xi]], writes=[rB[bA]], acc=(kc > 0))
                for kc in range(8):
                    S.op('pe', lambda e, kc=kc, c=c, bG=bG: e.matmul(bank(bG)[:, 0:ncol], lhsT=wag[:, kc, 512 + 128 * c:512 + 128 * c + 128], rhs=XT[:, xi, kc, xcols],
                                                                    start=(kc == 0), stop=(kc == 7)),
                         reads=[r_wag, rXT[xi]], writes=[rB[bG]], acc=(kc > 0))
                a = c % 2
                S.op('act', lambda e, a=a, bG=bG: e.activation(out=sg[a][:, 0:ncol], in_=bank(bG)[:, 0:ncol], func=AF.Sigmoid),
                     reads=[rB[bG]], writes=[r_sg[a]])
                S.op('dve', lambda e, a=a, bA=bA, c=c: e.tensor_tensor(out=uT[:, c, ucols], in0=bank(bA)[:, 0:ncol], in1=sg[a][:, 0:ncol], op=ALU.mult),
                     reads=[rB[bA], r_sg[a]], writes=[ru])
        glu(slice(480, 512), slice(0, 32), 32, r_uT[0], 3)
        for t in range(4):
            glu(slice(0, 512), slice(32 + 512 * t, 32 + 512 * t + 512), 512, r_uT[t + 1], 4 + t)
        dump("uT", uT, r_uT, [128, 4, TOK + 32], BF16)
        xs_z = xs_d.rearrange("(n r) d -> n r d", r=256)
        for n_ in range(NSLOT // 256):
            S.dma('sp', lambda e, n_=n_: e.dma_start(out=xs_z[n_], in_=zsrc), 'zxs', writes=[r_xs], nowaw=True)

        for t in range(4):
            ureads = [r_uT[t], r_uT[t + 1]] if t > 0 else [r_uT[0], r_uT[1]]
            for c in range(4):
                b = 4 + c % 2
                for j in range(31):
                    S.op('pe', lambda e, c=c, j=j, b=b, t=t: e.matmul(bank(b), lhsT=diag[:, c * 31 + j, :], rhs=uT[:, c, 512 * t + 2 + j:512 * t + 2 + j + 512],
                                                                     start=(j == 0), stop=(j == 30)),
                         reads=[r_diag] + ureads, writes=[rB[b]], acc=(j > 0))
                S.op('act', lambda e, c=c, b=b: e.activation(out=cT[:, c, :], in_=bank(b), func=AF.Identity, bias=cvt[:, c:c + 1]),
                     reads=[rB[b], r_cv], writes=[r_cT])
                S.op('act', lambda e, c=c: e.activation(out=csq[:, c, :], in_=cT[:, c, :], func=AF.Square),
                     reads=[r_cT], writes=[r_csq])
            if t == 0:
                dump("cT0", cT, [r_cT], [128, 4, 512])
            for c in range(4):
                S.op('pe', lambda e, c=c: e.matmul(bank(6), lhsT=onesf, rhs=cT[:, c, :], start=(c == 0), stop=(c == 3)),
                     reads=[r_cT, r_const], writes=[rB[6]], acc=(c > 0))
            for c in range(4):
                S.op('pe', lambda e, c=c: e.matmul(bank(7), lhsT=onesf, rhs=csq[:, c, :], start=(c == 0), stop=(c == 3)),
                     reads=[r_csq, r_const], writes=[rB[7]], acc=(c > 0))
            S.op('act', lambda e: e.activation(out=mean, in_=bank(6), func=AF.Copy, scale=1.0 / 512), reads=[rB[6]], writes=[r_st])
            S.op('dve', lambda e: e.tensor_tensor(out=msq, in0=mean, in1=mean, op=ALU.mult), reads=[r_st], writes=[r_st])
            S.op('dve', lambda e: e.scalar_tensor_tensor(out=var, in0=bank(7), scalar=1.0 / 512, in1=msq, op0=ALU.mult, op1=ALU.subtract),
                 reads=[rB[7], r_st], writes=[r_st])
            S.op('dve', lambda e: e.tensor_scalar(out=var, in0=var, scalar1=LN_EPS, scalar2=None, op0=ALU.add), reads=[r_st], writes=[r_st])
            S.op('act', lambda e: e.activation(out=var, in_=var, func=AF.Ln), reads=[r_st], writes=[r_st])
            S.op('act', lambda e: e.activation(out=rstd, in_=var, func=AF.Exp, scale=-0.5), reads=[r_st], writes=[r_st])
            for c in range(4):
                a = c % 2
                S.op('dve', lambda e, c=c, a=a: e.tensor_tensor(out=tn[a], in0=cT[:, c, :], in1=mean, op=ALU.subtract),
                     reads=[r_cT, r_st], writes=[r_tn[a]])
                S.op('pool', lambda e, a=a: e.tensor_tensor(out=tn[a], in0=tn[a], in1=rstd, op=ALU.mult),
                     reads=[r_tn[a], r_st], writes=[r_tn[a]])
                S.op('act', lambda e, c=c, a=a: e.activation(out=sT[:, c, :], in_=tn[a], func=AF.Silu, bias=cvt[:, 8 + c:9 + c], scale=cvt[:, 4 + c:5 + c]),
                     reads=[r_tn[a], r_cv], writes=[r_sT])
            for co in range(4):
                b = 2 * (co % 2)
                for ci in range(4):
                    S.op('pe', lambda e, co=co, ci=ci, b=b: e.matmul(bank(b), lhsT=pwb[:, ci, 128 * co:128 * co + 128], rhs=sT[:, ci, :],
                                                                    start=(ci == 0), stop=(ci == 3)),
                         reads=[r_pwb, r_sT], writes=[rB[b]], acc=(ci > 0))
                S.op('act', lambda e, co=co, b=b, t=t: e.activation(out=convoutT[:, co, 512 * t:512 * t + 512], in_=bank(b), func=AF.Identity,
                                                                   bias=cvt[:, 12 + co:13 + co]),
                     reads=[rB[b], r_cv], writes=[r_convout])
        dump("convoutT", convoutT, [r_convout], [128, 4, TOK], BF16)
        S.barrier()
        cur[0] = xt_mark
        IDX = alloc([64], I32)
        GK = alloc([64], F32)
        mark2 = cur[0]

        wob = alloc([8, DM], BF16)
        r_wob = S.res()
        lnv = alloc([4, DM], F32)
        r_lnv = S.res()
        rwt = alloc([8, NE], F32)
        rbt = alloc([NE], F32)
        r_rw = S.res()
        xtk = [alloc([DM], F32) for _ in range(3)]
        r_xtk = [S.res() for _ in range(3)]
        LG = alloc([16, NE], F32)
        T8 = alloc([16, 8], F32)
        MKB = alloc([16, NE], BF16)
        r_lg = [S.res() for _ in range(16)]
        assert cur[0] <= attnT_mark, cur[0]
        cur[0] = conv_mark
        yb = [alloc([DM], F32) for _ in range(3)]
        r_yb = [S.res() for _ in range(3)]
        x1b = [alloc([DM], F32) for _ in range(3)]
        r_x1 = [S.res() for _ in range(3)]
        X1H = alloc([16, DM], BF16)
        r_x1h = [S.res() for _ in range(16)]
        x1T = [alloc([8, 128], F32) for _ in range(2)]
        r_x1T = [S.res() for _ in range(2)]
        lnt = [(alloc([12], F32), alloc([2], F32), alloc([1], F32), alloc([1], F32), S.res()) for _ in range(2)]
        DSTB = alloc([8, NE], F32)
        OH4 = alloc([8, 4, NE], F32)
        DK = alloc([32], F32)
        D4 = alloc([32], F32)
        S4 = alloc([8], F32)
        r_bt = S.res()
        r_x1d = [S.res() for _ in range(16)]

        S.dma('pool', lambda e: e.dma_start(out=wob, in_=w_out.rearrange("(k p) n -> p k n", p=128)), 'ldwo', writes=[r_wob])
        S.dma('sp', lambda e: e.dma_start(out=lnv, in_=lnvec.rearrange("p (a b) -> p a b", b=DM)), 'ldln', writes=[r_lnv])
        S.dma('sp', lambda e: e.dma_start(out=rwt, in_=rw.rearrange("(k p) n -> p k n", p=128)), 'ldrw', writes=[r_rw])
        S.dma('sp', lambda e: e.dma_start(out=rbt, in_=rbr), 'ldrw', writes=[r_rw], nowaw=True)

        catT = [convoutT[:, c, :] for c in range(4)] + [attnT[:, c, :] for c in range(4)]

        def fap(base, off, free):
            return bass.AP(base.tensor, base.offset + off, [list(base.ap[0])] + [list(x) for x in free])

        def ln_apply(src, rsrc, dsts, rdsts, gi, tmps, lnv, r_lnv, gb_eng='pool', part=0):
            bst, mv, rs1, nmr, r_ln = tmps
            if part == 2:
                S.op(gb_eng, lambda e: e.tensor_tensor(out=src, in0=src, in1=lnv[:, gi, :], op=ALU.mult), reads=[rsrc, r_lnv], writes=[rsrc])
                S.op(gb_eng, lambda e: e.tensor_tensor(out=dsts, in0=src, in1=lnv[:, gi + 1, :], op=ALU.add), reads=[rsrc, r_lnv], writes=[rdsts])
                return
            S.op('dve', lambda e: e.bn_stats(out=bst[:, 0:6], in_=src[:, 0:512]), reads=[rsrc], writes=[r_ln])
            S.op('dve', lambda e: e.bn_stats(out=bst[:, 6:12], in_=src[:, 512:1024]), reads=[rsrc], writes=[r_ln])
            S.op('dve', lambda e: e.bn_aggr(out=mv, in_=bst), reads=[r_ln], writes=[r_ln])
            S.op('dve', lambda e: e.tensor_scalar(out=rs1, in0=mv[:, 1:2], scalar1=LN_EPS, scalar2=None, op0=ALU.add), reads=[r_ln], writes=[r_ln])
            S.op('act', lambda e: e.activation(out=rs1, in_=rs1, func=AF.Sqrt), reads=[r_ln], writes=[r_ln])
            S.op('dve', lambda e: e.reciprocal(out=rs1, in_=rs1), reads=[r_ln], writes=[r_ln])
            S.op('dve', lambda e: e.tensor_scalar(out=nmr, in0=mv[:, 0:1], scalar1=rs1, scalar2=-1.0, op0=ALU.mult, op1=ALU.mult),
                 reads=[r_ln], writes=[r_ln])
            S.op('act', lambda e: e.activation(out=src, in_=src, func=AF.Identity, bias=nmr, scale=rs1), reads=[rsrc, r_ln], writes=[rsrc])
            if part == 1:
                return
            S.op(gb_eng, lambda e: e.tensor_tensor(out=src, in0=src, in1=lnv[:, gi, :], op=ALU.mult), reads=[rsrc, r_lnv], writes=[rsrc])
            S.op(gb_eng, lambda e: e.tensor_tensor(out=dsts, in0=src, in1=lnv[:, gi + 1, :], op=ALU.add), reads=[rsrc, r_lnv], writes=[rdsts])

        def stage1(i):
            a = i % 3
            tsl = slice(128 * i, 128 * i + 128)
            S.dma('sp', lambda e, a=a, tsl=tsl: e.dma_start(out=xtk[a], in_=xtok[tsl, :]), f'ldxk{a}', writes=[r_xtk[a]])
            for h_ in range(2):
                b = 2 * (i % 2) + h_
                for fc in range(8):
                    S.op('pe', lambda e, fc=fc, b=b, h_=h_, tsl=tsl: e.matmul(bank(b), lhsT=catT[fc][:, tsl], rhs=wob[:, fc, 512 * h_:512 * h_ + 512],
                                                                             start=(fc == 0), stop=(fc == 7)),
                         reads=[r_convout, r_wob] + r_attnT, writes=[rB[b]], acc=(fc > 0))
                S.op('dve', lambda e, a=a, b=b, h_=h_: e.scalar_tensor_tensor(out=yb[a][:, 512 * h_:512 * h_ + 512], in0=xtk[a][:, 512 * h_:512 * h_ + 512],
                                                                             scalar=ALPHA, in1=bank(b), op0=ALU.mult, op1=ALU.add),
                     reads=[r_xtk[a], rB[b]], writes=[r_yb[a]])
            if i == 0:
                dump("y0", yb[0], [r_yb[0]], [128, DM])
            ln_apply(yb[a], r_yb[a], x1b[a], r_x1[a], 0, lnt[i % 2], lnv, r_lnv, gb_eng='dve', part=1)

        def stage1b(i):
            a = i % 3
            tsl = slice(128 * i, 128 * i + 128)
            ln_apply(yb[a], r_yb[a], x1b[a], r_x1[a], 0, lnt[i % 2], lnv, r_lnv, gb_eng='dve', part=2)
            S.dma('sp', lambda e, a=a, tsl=tsl: e.dma_start(out=x1_d[tsl, :], in_=x1b[a]), f'stx1{a}', reads=[r_x1[a]], writes=[r_x1d[i]])
            S.op('act', lambda e, a=a, i=i: e.copy(out=X1H[:, i, :], in_=x1b[a]), reads=[r_x1[a]], writes=[r_x1h[i]])
            if i == 0:
                dump("x10", x1b[0], [r_x1[0]], [128, DM])

        def stage2(i):
            a = i % 3
            a2 = i % 2
            for kc in range(8):
                b = 4 + kc // 4
                S.op('pe', lambda e, kc=kc, b=b, a=a: e.transpose(bank(b)[:, 128 * (kc % 4):128 * (kc % 4) + 128], x1b[a][:, 128 * kc:128 * kc + 128], identf),
                     reads=[r_x1[a], r_const], writes=[rB[b]], acc=(kc % 4 > 0))
            for h_ in range(2):
                S.op('act' if h_ == 0 else 'dve',
                     (lambda e, a2=a2, h_=h_: e.copy(out=x1T[a2][:, 4 * h_:4 * h_ + 4, :], in_=bank(4 + h_).rearrange("p (k t) -> p k t", t=128))) if h_ == 0 else
                     (lambda e, a2=a2, h_=h_: e.tensor_copy(out=x1T[a2][:, 4 * h_:4 * h_ + 4, :], in_=bank(4 + h_).rearrange("p (k t) -> p k t", t=128))),
                     reads=[rB[4 + h_]], writes=[r_x1T[a2]])

        def stage2b(i):
            a2 = i % 2
            for kc in range(8):
                S.op('pe', lambda e, kc=kc, a2=a2: e.matmul(bank(6)[:, 0:NE], lhsT=x1T[a2][:, kc, :], rhs=rwt[:, kc, :], start=(kc == 0), stop=(kc == 7)),
                     reads=[r_x1T[a2], r_rw], writes=[rB[6]], acc=(kc > 0))
            S.op('dve', lambda e, i=i: e.tensor_tensor(out=LG[:, i, :], in0=bank(6)[:, 0:NE], in1=rbt, op=ALU.add), reads=[rB[6], r_rw], writes=[r_lg[i]])
            S.op('dve', lambda e, i=i: e.max(out=T8[:, i, :], in_=LG[:, i, :]), reads=[r_lg[i]], writes=[r_lg[i]])
            S.op('dve', lambda e, i=i: e.tensor_scalar(out=MKB[:, i, :], in0=LG[:, i, :], scalar1=T8[:, i, 3:4], scalar2=None, op0=ALU.is_ge),
                 reads=[r_lg[i]], writes=[r_lg[i]])

        def batch(bt):
            i0_ = 8 * bt
            for il in range(8):
                i = i0_ + il
                oc = bank(7)[:, 32 * il:32 * il + 32]
                S.op('pe', lambda e, i=i, oc=oc: e.matmul(oc, lhsT=trib, rhs=MKB[:, i, :], start=True, stop=(i == 0), skip_group_check=True),
                     reads=[r_lg[i], r_const], writes=[rB[7]], acc=(il > 0))
                for j in range(i):
                    S.op('pe', lambda e, j=j, i=i, oc=oc: e.matmul(oc, lhsT=onesb, rhs=MKB[:, j, :], start=False, stop=(j == i - 1), skip_group_check=True),
                         reads=[r_lg[j], r_const], writes=[rB[7]], acc=True)
            R = r_bt
            lgs = [r_lg[i0_ + il] for il in range(8)]
            S.op('dve', lambda e: e.tensor_tensor(out=DSTB, in0=bank(7)[:, 0:256].rearrange("p (t n) -> p t n", n=NE),
                                                  in1=fap(ecf, 0, [[0, 8], [1, NE]]), op=ALU.add),
                 reads=[rB[7], r_const], writes=[R])
            S.op('dve', lambda e: e.tensor_tensor(out=OH4, in0=fap(LG, i0_ * NE, [[NE, 8], [0, 4], [1, NE]]),
                                                  in1=fap(T8, i0_ * 8, [[8, 8], [1, 4], [0, NE]]), op=ALU.is_equal),
                 reads=lgs, writes=[R])
            S.op('dve', lambda e: e.tensor_tensor(out=OH4, in0=OH4, in1=fap(DSTB, 0, [[NE, 8], [0, 4], [1, NE]]), op=ALU.mult),
                 reads=[R], writes=[R])
            S.op('dve', lambda e: e.tensor_reduce(out=DK, in_=OH4.rearrange("p t k n -> p (t k) n"), axis=AX.X, op=ALU.add), reads=[R], writes=[R])
            S.op('dve', lambda e: e.tensor_copy(out=IDX[:, 32 * bt:32 * bt + 32], in_=DK), reads=[R], writes=[r_idx])
            S.op('dve', lambda e: e.tensor_tensor(out=D4.rearrange("p (t k) -> p t k", k=4), in0=fap(T8, i0_ * 8, [[8, 8], [1, 4]]),
                                                  in1=fap(T8, i0_ * 8, [[8, 8], [0, 4]]), op=ALU.subtract),
                 reads=lgs, writes=[R])
            S.op('act', lambda e: e.activation(out=D4, in_=D4, func=AF.Exp), reads=[R], writes=[R])
            S.op('dve', lambda e: e.tensor_reduce(out=S4, in_=D4.rearrange("p (t k) -> p t k", k=4), axis=AX.X, op=ALU.add), reads=[R], writes=[R])
            S.op('dve', lambda e: e.reciprocal(out=S4, in_=S4), reads=[R], writes=[R])
            S.op('dve', lambda e: e.tensor_tensor(out=GK[:, 32 * bt:32 * bt + 32].rearrange("p (t k) -> p t k", k=4),
                                                  in0=D4.rearrange("p (t k) -> p t k", k=4), in1=fap(S4, 0, [[1, 8], [0, 4]]), op=ALU.mult),
                 reads=[R], writes=[r_idx])
            for il in range(8):
                i = i0_ + il
                for k in range(4):
                    S.dma('pool', lambda e, k=k, i=i: e.indirect_dma_start(
                        out=xs_d, out_offset=bass.IndirectOffsetOnAxis(ap=IDX[:, 4 * i + k:4 * i + k + 1], axis=0),
                        in_=X1H[:, i, :], in_offset=None, bounds_check=bcreg(e), oob_is_err=False),
                        f'scat{bt}', reads=[r_x1h[i], r_idx], writes=[r_xs], nowaw='any')

        stage1(0)
        stage1b(0)
        stage1(1)
        stage1b(1)
        for i in range(16):
            if i + 2 < 16:
                stage1(i + 2)
            stage2(i)
            if i + 2 < 16:
                stage1b(i + 2)
            stage2b(i)
            if i == 7:
                batch(0)
        batch(1)
        dump("LG", LG, r_lg, [128, 16, NE])
        dump("IDX", IDX, [r_idx], [128, 64], I32)
        dump("GK", GK, [r_idx], [128, 64])
        S.barrier()
        cur[0] = mark2
        if stop == 'router':
            final = []
            for k, v in S.cnt.items():
                if k.startswith('dbg'):
                    final.append(('d', k, v, 'sp'))
            S.wait_all('sp', final)
            S.emit()
            return nc, dbg_out

        NRING = 8
        Wr = [alloc([8, DM], BF16) for _ in range(NRING)]
        r_Wr = [S.res() for _ in range(NRING)]
        bgt = alloc([2 * NE * 8], F32)
        bu1 = alloc([NE * 8], F32)
        r_bg = S.res()
        xsb = [alloc([3, DM], BF16) for _ in range(2)]
        r_xsb = [S.res() for _ in range(2)]
        xsT = [alloc([8, CAP], BF16) for _ in range(2)]
        r_xsT = [S.res() for _ in range(2)]
        hT = alloc([8, CAP], BF16)
        r_hT = S.res()
        gc = [alloc([CAP], F32) for _ in range(2)]
        sgm = [alloc([CAP], F32) for _ in range(2)]
        u1 = [alloc([CAP], F32) for _ in range(2)]
        r_ew = [S.res() for _ in range(2)]
        yo = [alloc([DM], F32) for _ in range(3)]
        r_yo = [S.res() for _ in range(3)]
        r_ys = S.res("ys_scr")
        bdb = [alloc([DM], F32) for _ in range(2)]
        r_bdb = [S.res() for _ in range(2)]

        def load_bd(e_):
            s_ = e_ % 2
            S.dma('sp', lambda e, s_=s_, e_=e_: e.dma_start(out=bdb[s_], in_=bd_d[e_:e_ + 1, :].partition_broadcast(128)), f'ldbd{s_}', writes=[r_bdb[s_]])

        wsrc = [wg_d.rearrange("(e p) f -> p e f", p=128), wu_d.rearrange("(e p) f -> p e f", p=128),
                wd_d.rearrange("(e p) f -> p e f", p=128)]
        xs_v = xs_d.rearrange("(e s p) d -> p e s d", p=128, s=3)
        ycount = [0]

        def load_mat(n):
            if n >= 3 * NE:
                return
            e_, m, sl_ = n // 3, n % 3, n % NRING
            S.dma('pool', lambda e, m=m, sl_=sl_, e_=e_: e.dma_start(out=Wr[sl_].rearrange("p k n -> p (k n)"), in_=wsrc[m][:, e_], max_dma_last_dim=4096), f'ldW{sl_}', writes=[r_Wr[sl_]])

        def load_xs(e_):
            s_ = e_ % 2
            S.dma('sp', lambda e, s_=s_, e_=e_: e.dma_start(out=xsb[s_], in_=xs_v[:, e_]), f'ldxs{s_}', reads=[r_xs], writes=[r_xsb[s_]])

        def transposes(e_):
            s_ = e_ % 2
            for stl in range(3):
                b = stl % 2
                for kc in range(8):
                    S.op('pe', lambda e, b=b, kc=kc, stl=stl, s_=s_: e.transpose(bankb(b)[:, 128 * kc:128 * kc + 128], xsb[s_][:, stl, 128 * kc:128 * kc + 128], identb),
                         reads=[r_xsb[s_], r_const], writes=[rB[b]], acc=(kc > 0))
                S.op('act' if stl != 1 else 'dve',
                     (lambda e, b=b, stl=stl, s_=s_: e.copy(out=xsT[s_][:, :, 128 * stl:128 * stl + 128], in_=bankb(b).rearrange("p (k t) -> p k t", t=128))) if stl != 1 else
                     (lambda e, b=b, stl=stl, s_=s_: e.tensor_copy(out=xsT[s_][:, :, 128 * stl:128 * stl + 128], in_=bankb(b).rearrange("p (k t) -> p k t", t=128))),
                     reads=[rB[b]], writes=[r_xsT[s_]])

        for n in range(NRING):
            load_mat(n)
        S.dma('sp', lambda e: e.dma_start(out=bgt, in_=bgu), 'ldbg', writes=[r_bg])
        S.op('dve', lambda e: e.tensor_scalar(out=bu1, in0=bgt[:, NE * 8:2 * NE * 8], scalar1=1.0, scalar2=None, op0=ALU.add), reads=[r_bg], writes=[r_bg])
        load_xs(0)
        load_xs(1)
        load_bd(0)
        load_bd(1)
        transposes(0)
        for e_ in range(NE):
            s_ = e_ % 2
            Wg_, Wu_, Wd_ = Wr[(3 * e_) % NRING], Wr[(3 * e_ + 1) % NRING], Wr[(3 * e_ + 2) % NRING]
            rWg, rWu, rWd = r_Wr[(3 * e_) % NRING], r_Wr[(3 * e_ + 1) % NRING], r_Wr[(3 * e_ + 2) % NRING]
            if e_ == 0:
                dump("xsT0", xsT[0], [r_xsT[0]], [128, 8, CAP], BF16)
            for fc in range(8):
                a = fc % 2
                bG, bU = 2 + 2 * a, 3 + 2 * a
                for kc in range(8):
                    S.op('pe', lambda e, kc=kc, fc=fc, bG=bG, s_=s_, Wg_=Wg_: e.matmul(bank(bG)[:, 0:CAP], lhsT=Wg_[:, kc, 128 * fc:128 * fc + 128], rhs=xsT[s_][:, kc, :],
                                                                                      start=(kc == 0), stop=(kc == 7)),
                         reads=[rWg, r_xsT[s_]], writes=[rB[bG]], acc=(kc > 0))
                for kc in range(8):
                    S.op('pe', lambda e, kc=kc, fc=fc, bU=bU, s_=s_, Wu_=Wu_: e.matmul(bank(bU)[:, 0:CAP], lhsT=Wu_[:, kc, 128 * fc:128 * fc + 128], rhs=xsT[s_][:, kc, :],
                                                                                      start=(kc == 0), stop=(kc == 7)),
                         reads=[rWu, r_xsT[s_]], writes=[rB[bU]], acc=(kc > 0))
                bi = e_ * 8 + fc
                S.op('dve', lambda e, a=a, bG=bG, bi=bi: e.tensor_scalar(out=gc[a], in0=bank(bG)[:, 0:CAP], scalar1=bgt[:, bi:bi + 1], scalar2=7.0,
                                                                        op0=ALU.add, op1=ALU.min),
                     reads=[rB[bG], r_bg], writes=[r_ew[a]])
                S.op('act', lambda e, a=a: e.activation(out=sgm[a], in_=gc[a], func=AF.Silu, scale=1.702), reads=[r_ew[a]], writes=[r_ew[a]])
                S.op('dve', lambda e, a=a, bU=bU, bi=bi: e.tensor_scalar(out=u1[a], in0=bank(bU)[:, 0:CAP], scalar1=bu1[:, bi:bi + 1], scalar2=8.0,
                                                                        op0=ALU.add, op1=ALU.min),
                     reads=[rB[bU], r_bg], writes=[r_ew[a]])
                S.op('dve', lambda e, a=a, fc=fc: e.scalar_tensor_tensor(out=hT[:, fc, :], in0=u1[a], scalar=-6.0, in1=sgm[a], op0=ALU.max, op1=ALU.mult),
                     reads=[r_ew[a]], writes=[r_hT])
            load_mat(3 * e_ + NRING)
            load_mat(3 * e_ + 1 + NRING)
            if e_ == 0:
                dump("hT0", hT, [r_hT], [128, 8, CAP], BF16)
            if e_ + 1 < NE:
                transposes(e_ + 1)
            if e_ + 2 < NE:
                load_xs(e_ + 2)
            for stl in range(3):
                yi = ycount[0] % 3
                ycount[0] += 1
                for h_ in range(2):
                    b = 6 + h_
                    for fc in range(8):
                        S.op('pe', lambda e, fc=fc, b=b, h_=h_, stl=stl, Wd_=Wd_: e.matmul(bank(b), lhsT=hT[:, fc, 128 * stl:128 * stl + 128],
                                                                                          rhs=Wd_[:, fc, 512 * h_:512 * h_ + 512],
                                                                                          start=(fc == 0), stop=(fc == 7)),
                             reads=[r_hT, rWd], writes=[rB[b]], acc=(fc > 0))
                    S.op('dve', lambda e, b=b, h_=h_, yi=yi, s_=s_: e.scalar_tensor_tensor(out=yo[yi][:, 512 * h_:512 * h_ + 512], in0=bank(b), scalar=1.0 / 1.702,
                                                                                          in1=bdb[s_][:, 512 * h_:512 * h_ + 512], op0=ALU.mult, op1=ALU.add),
                         reads=[rB[b], r_bdb[s_]], writes=[r_yo[yi]])
                if e_ == 0 and stl == 0:
                    dump("yo0", yo[yi], [r_yo[yi]], [128, DM])
                row0 = e_ * CAP + 128 * stl
                S.dma('sp', lambda e, yi=yi, row0=row0: e.dma_start(out=ys_d[row0:row0 + 128, :], in_=yo[yi]), f'sty{yi}',
                      reads=[r_yo[yi]], writes=[r_ys], nowaw='any')
            load_mat(3 * e_ + 2 + NRING)
            if e_ + 2 < NE:
                load_bd(e_ + 2)
        S.barrier()
        cur[0] = mark2

        lnv2 = alloc([4, DM], F32)
        r_lnv2 = S.res()
        x1c = [alloc([DM], F32) for _ in range(3)]
        r_x1c = [S.res() for _ in range(3)]
        yk = [[alloc([DM], F32) for _ in range(4)] for _ in range(3)]
        r_yk = [[S.res() for _ in range(4)] for _ in range(3)]
        acc_ = [alloc([DM], F32) for _ in range(2)]
        r_acc = [S.res() for _ in range(2)]
        ob_ = [alloc([DM], F32) for _ in range(2)]
        r_ob = [S.res() for _ in range(2)]
        lnt2 = [(alloc([12], F32), alloc([2], F32), alloc([1], F32), alloc([1], F32), S.res()) for _ in range(2)]
        S.dma('sp', lambda e: e.dma_start(out=lnv2, in_=lnvec.rearrange("p (a b) -> p a b", b=DM)), 'ldln2', writes=[r_lnv2])
        out_toks = []

        def fetch(i):
            c3 = i % 3
            tsl = slice(128 * i, 128 * i + 128)
            S.dma('sp', lambda e, c3=c3, tsl=tsl: e.dma_start(out=x1c[c3], in_=x1_d[tsl, :]), f'ldx1{c3}', reads=[r_x1d[i]], writes=[r_x1c[c3]])
            for k in range(4):
                S.dma('pool', lambda e, c3=c3, k=k, i=i: e.indirect_dma_start(
                    out=yk[c3][k], out_offset=None, in_=ys_d,
                    in_offset=bass.IndirectOffsetOnAxis(ap=IDX[:, 4 * i + k:4 * i + k + 1], axis=0),
                    bounds_check=bcreg(e), oob_is_err=False),
                    f'gat{c3}{k}', reads=[r_ys, r_idx], writes=[r_yk[c3][k]])

        def accum(i):
            a = i % 2
            c3 = i % 3
            S.op('act', lambda e, a=a, c3=c3: e.activation(out=acc_[a], in_=x1c[c3], func=AF.Copy, scale=ALPHA), reads=[r_x1c[c3]], writes=[r_acc[a]])

        def accum2(i):
            a = i % 2
            c3 = i % 3
            for k in range(4):
                S.op('dve', lambda e, a=a, k=k, i=i, c3=c3: e.scalar_tensor_tensor(out=acc_[a], in0=yk[c3][k], scalar=GK[:, 4 * i + k:4 * i + k + 1], in1=acc_[a],
                                                                                  op0=ALU.mult, op1=ALU.add),
                     reads=[r_yk[c3][k], r_idx, r_acc[a]], writes=[r_acc[a]])

        def ln2_stats(i):
            a = i % 2
            src, rsrc = acc_[a], r_acc[a]
            bst, mv, rs1, nmr, r_ln = lnt2[i % 2]
            S.op('dve', lambda e: e.bn_stats(out=bst[:, 0:6], in_=src[:, 0:512]), reads=[rsrc], writes=[r_ln])
            S.op('dve', lambda e: e.bn_stats(out=bst[:, 6:12], in_=src[:, 512:1024]), reads=[rsrc], writes=[r_ln])
            S.op('dve', lambda e: e.bn_aggr(out=mv, in_=bst), reads=[r_ln], writes=[r_ln])
            S.op('dve', lambda e: e.tensor_scalar(out=rs1, in0=mv[:, 1:2], scalar1=LN_EPS, scalar2=None, op0=ALU.add), reads=[r_ln], writes=[r_ln])
            S.op('act', lambda e: e.activation(out=rs1, in_=rs1, func=AF.Sqrt), reads=[r_ln], writes=[r_ln])
            S.op('dve', lambda e: e.reciprocal(out=rs1, in_=rs1), reads=[r_ln], writes=[r_ln])
            S.op('dve', lambda e: e.tensor_scalar(out=nmr, in0=mv[:, 0:1], scalar1=rs1, scalar2=-1.0, op0=ALU.mult, op1=ALU.mult),
                 reads=[r_ln], writes=[r_ln])
            S.op('act', lambda e: e.activation(out=src, in_=src, func=AF.Identity, bias=nmr, scale=rs1), reads=[rsrc, r_ln], writes=[rsrc])

        def ln2_gb(i):
            a = i % 2
            tsl = slice(128 * i, 128 * i + 128)
            src, rsrc = acc_[a], r_acc[a]
            S.op('dve', lambda e: e.tensor_tensor(out=src, in0=src, in1=lnv2[:, 2, :], op=ALU.mult), reads=[rsrc, r_lnv2], writes=[rsrc])
            S.op('dve', lambda e: e.tensor_tensor(out=ob_[a], in0=src, in1=lnv2[:, 3, :], op=ALU.add), reads=[rsrc, r_lnv2], writes=[r_ob[a]])
            out_toks.append(S.dma('sp', lambda e, a=a, tsl=tsl: e.dma_start(out=out_d[tsl, :], in_=ob_[a]), f'sto{a}', reads=[r_ob[a]]))

        fetch(0)
        fetch(1)
        fetch(2)
        accum(0)
        accum2(0)
        for i in range(16):
            if i + 3 < 16:
                fetch(i + 3)
            if i + 1 < 16:
                accum(i + 1)
            ln2_stats(i)
            if i + 1 < 16:
                accum2(i + 1)
            ln2_gb(i)
        final = [t for t in out_toks]
        for k, v in S.cnt.items():
            if k.startswith('dbg'):
                final.append(('d', k, v, 'sp'))
        S.wait_all('sp', final)
        S.emit()
    return nc, dbg_out


def _inv_freq():
    e = 64
    try:
        import jax
        import jax.numpy as jnp
        with jax.default_device(jax.devices('cpu')[0]):
            v = 10000.0 ** (-jnp.arange(0, e, 2, dtype=jnp.float32) / e)
            return np.asarray(v, dtype=np.float32)
    except Exception:
        return (np.float32(10000.0) ** (-np.arange(0, e, 2, dtype=np.float32) / np.float32(e))).astype(np.float32)


def make_in_maps(x, positions, w_in, conv_w, conv_b, conv_ln_g, conv_ln_b, conv_pw_w, conv_pw_b, w_out, ln1_g, ln1_b,
                 router_w, router_b, exp_w_gate, exp_b_gate, exp_w_up, exp_b_up, exp_w_down, exp_b_down, ln2_g, ln2_b):
    f = lambda a: np.ascontiguousarray(np.asarray(a, dtype=np.float32))
    x = f(x)
    positions = np.asarray(positions).astype(np.int32)

    def p4(v):
        return f(v).reshape(4, 128).T
    cvec = np.ascontiguousarray(np.concatenate([p4(conv_b[0]), p4(conv_ln_g[0]), p4(conv_ln_b[0]), p4(conv_pw_b[0])], axis=1))
    convw = np.ascontiguousarray(f(conv_w[0]).T.reshape(4, 128, 31).transpose(1, 0, 2).reshape(128, 124))
    lnvec = np.ascontiguousarray(np.broadcast_to(np.concatenate([f(ln1_g[0]), f(ln1_b[0]), f(ln2_g[0]), f(ln2_b[0])])[None, :], (128, 4 * DM)))
    rbr = np.ascontiguousarray(np.broadcast_to(f(router_b[0])[None, :], (128, NE)))
    bg = f(exp_b_gate[0]).reshape(NE, 8, 128).transpose(2, 0, 1).reshape(128, NE * 8)
    bu = f(exp_b_up[0]).reshape(NE, 8, 128).transpose(2, 0, 1).reshape(128, NE * 8)
    bgu = np.ascontiguousarray(np.concatenate([bg, bu], axis=1))
    invf = _inv_freq()
    pidx = np.arange(128)

    def wtile(w):
        return np.ascontiguousarray(f(w).reshape(NE, 8, 128, DM).transpose(0, 2, 1, 3)).reshape(NE * 128, 8 * DM)
    shared = dict(
        w_in=f(w_in[0]), convw=convw, cvec=cvec, pw_w=f(conv_pw_w[0]), w_out=f(w_out[0]), lnvec=lnvec,
        rw=f(router_w[0]), rbr=rbr, wg=wtile(exp_w_gate[0]), wu=wtile(exp_w_up[0]),
        wd=wtile(exp_w_down[0]), bgu=bgu, bd=f(exp_b_down[0]),
        zsrc=np.zeros((256, DM), dtype=ml_dtypes.bfloat16),
    )
    maps = []
    for c in range(NCORES):
        b, qi = c // 4, c % 4
        t0 = TOK * qi
        xTc = np.zeros((DM, TT), np.float32)
        posc = np.zeros((1, TT), np.int32)
        if qi > 0:
            xTc[:, :TOK] = x[b, t0 - TOK:t0].T
            posc[0, :TOK] = positions[b, t0 - TOK:t0]
        xTc[:, TOK:] = x[b, t0:t0 + TOK].T
        posc[0, TOK:] = positions[b, t0:t0 + TOK]
        cstc = np.zeros((128, 4), np.float32)
        cstc[:, 0] = invf[pidx % 32]
        cstc[:, 1] = np.where((pidx % 64) < 32, -1.0, 1.0)
        cstc[:, 2] = 1.0 if qi > 0 else 0.0
        m = dict(shared)
        xTc = np.ascontiguousarray(xTc.reshape(8, 128, 8, 512).transpose(2, 1, 0, 3)).reshape(8, 128, 8 * 512)
        m.update(xT=xTc, xtok=np.ascontiguousarray(x[b, t0:t0 + TOK]), pos=posc, cst=cstc)
        maps.append(m)
    return maps


_NC_CACHE = {}


def kernel(**inputs):
    if 'nc' not in _NC_CACHE:
        _NC_CACHE['nc'] = build_program()[0]
    nc = _NC_CACHE['nc']
    in_maps = make_in_maps(**inputs)
    res = run_bass_kernel_spmd(nc, in_maps, core_ids=list(range(NCORES)))
    out = np.zeros((2, 8192, DM), np.float32)
    for c in range(NCORES):
        b, qi = c // 4, c % 4
        out[b, TOK * qi:TOK * qi + TOK] = np.asarray(res.results[c]["out"], dtype=np.float32)
    return out
```
